# Optimizing a Trainium2 kernel written in Bass

```python
import jax
import jax.numpy as jnp
from jax import lax
import numpy as np

D_MODEL = 1024
BATCH = 8
SEQ = 2048
DEPTH = 1

MIX_WIDTH = D_MODEL
HEAD_DIM = 64
NSA_WIDTH = MIX_WIDTH // 2
CONV_WIDTH = MIX_WIDTH - NSA_WIDTH
NSA_HEADS = NSA_WIDTH // HEAD_DIM
NSA_KV_HEADS = 2
GQA_REP = NSA_HEADS // NSA_KV_HEADS
N_NSA_BRANCH = 3
CMP_LEN = 32
CMP_STRIDE = 16
SEL_LEN = 64
SEL_TOPN = 16
WINDOW = 512
Q_BLOCK = 64
FORCE_BONUS = 1.0e4
CONV_TAPS = 31
PEER_HEADS = 8
PEER_NKEYS = 128
PEER_EXPERTS = PEER_NKEYS * PEER_NKEYS
PEER_QDIM = 256
PEER_TOPK = 16
PEER_TOK_BLOCK = 128
NORM_EPS = 1e-6
NEG_INF = -1e30

N_Q_COLS = NSA_HEADS * HEAD_DIM
N_KV_COLS = 2 * N_NSA_BRANCH * NSA_KV_HEADS * HEAD_DIM
N_GATE_COLS = NSA_HEADS * N_NSA_BRANCH
N_GLU_COLS = 2 * CONV_WIDTH
IN_COLS = N_Q_COLS + N_KV_COLS + N_GATE_COLS + N_GLU_COLS

kernel_name = 'hybrid_nsa_conformer_peer_layer'


def rmsnorm(x, g):
    xf = x.astype(jnp.float32)
    y = xf * lax.rsqrt(jnp.mean(xf * xf, axis=-1, keepdims=True) + NORM_EPS)
    return (y * g).astype(x.dtype)


def modulate(h, shift, scale):
    return h * (1.0 + scale[:, None, :]) + shift[:, None, :]


def masked_softmax(s, mask):
    p = jax.nn.softmax(jnp.where(mask, s, NEG_INF), axis=-1)
    return jnp.where(mask, p, 0.0)


def alibi_slopes():
    h = np.arange(1, NSA_HEADS + 1, dtype=np.float32)
    return (2.0 ** (-8.0 * h / NSA_HEADS)).astype(np.float32).reshape(NSA_KV_HEADS, GQA_REP)


def nsa_mixer(q, kv, gate_logits, cmp_pe_k, cmp_pe_v, w_cmp_k, w_cmp_v, qk_norm_g):
    B, S = q.shape[0], q.shape[1]
    G, R, hd = NSA_KV_HEADS, GQA_REP, HEAD_DIM
    slopes = jnp.asarray(alibi_slopes())
    t_pos = np.arange(S)
    q = rmsnorm(q, qk_norm_g[0]) * (hd ** -0.5)
    q = q.reshape(B, S, G, R, hd).transpose(0, 2, 3, 1, 4)
    kv = kv.transpose(2, 0, 3, 1, 4)
    k_cmp, v_cmp, k_slc, v_slc, k_win, v_win = kv[0], kv[1], kv[2], kv[3], kv[4], kv[5]

    n_cmp = (S - CMP_LEN) // CMP_STRIDE + 1
    blk_tok = np.arange(n_cmp)[:, None] * CMP_STRIDE + np.arange(CMP_LEN)[None, :]
    kc = jnp.einsum('bgnld,lde->bgne', k_cmp[:, :, blk_tok] + cmp_pe_k, w_cmp_k)
    vc = jnp.einsum('bgnld,lde->bgne', v_cmp[:, :, blk_tok] + cmp_pe_v, w_cmp_v)
    kc = rmsnorm(kc, qk_norm_g[1])
    blk_end = blk_tok[:, -1]
    cdist = (t_pos[:, None] - blk_end[None, :]).astype(np.float32)
    s_cmp = jnp.einsum('bgrtd,bgnd->bgrtn', q, kc).astype(jnp.float32) - slopes[:, :, None, None] * cdist
    p_cmp = masked_softmax(s_cmp, cdist >= 0)
    o_cmp = jnp.einsum('bgrtn,bgnd->bgrtd', p_cmp.astype(vc.dtype), vc)

    n_sel = S // SEL_LEN
    top_n = min(SEL_TOPN, n_sel)
    sel_start = np.arange(n_sel) * SEL_LEN
    overlap = np.clip(np.minimum(blk_tok[:, -1:] + 1, sel_start[None, :] + SEL_LEN)
                      - np.maximum(blk_tok[:, :1], sel_start[None, :]), 0, None).astype(np.float32) / CMP_LEN
    imp = jnp.einsum('bgrtn,nj->bgtj', p_cmp, overlap)
    t_blk = t_pos // SEL_LEN
    j = np.arange(n_sel)
    valid = j[None, :] <= t_blk[:, None]
    forced = ((j[None, :] == 0) | (j[None, :] == t_blk[:, None]) | (j[None, :] == t_blk[:, None] - 1)).astype(np.float32)
    imp = jnp.where(valid, imp + FORCE_BONUS * forced, -1.0)
    _, sel_idx = lax.top_k(imp, top_n)

    ks_blocks = rmsnorm(k_slc, qk_norm_g[2]).reshape(B, G, n_sel, SEL_LEN, hd)
    vs_blocks = v_slc.reshape(B, G, n_sel, SEL_LEN, hd)
    kw_pad = jnp.pad(rmsnorm(k_win, qk_norm_g[3]), ((0, 0), (0, 0), (WINDOW, 0), (0, 0)))
    vw_pad = jnp.pad(v_win, ((0, 0), (0, 0), (WINDOW, 0), (0, 0)))
    n_q = S // Q_BLOCK
    q_chunks = q.reshape(B, G, R, n_q, Q_BLOCK, hd).transpose(3, 0, 1, 2, 4, 5)
    idx_chunks = sel_idx.reshape(B, G, n_q, Q_BLOCK, top_n).transpose(2, 0, 1, 3, 4)
    b_ix = jnp.arange(B)[:, None, None, None]
    g_ix = jnp.arange(G)[None, :, None, None]
    m_sel = top_n * SEL_LEN

    def block_fn(args):
        ci, qc, ic = args
        t = ci * Q_BLOCK + jnp.arange(Q_BLOCK)
        ks = ks_blocks[b_ix, g_ix, ic]
        vs = vs_blocks[b_ix, g_ix, ic]
        spos = ic[..., None] * SEL_LEN + jnp.arange(SEL_LEN)
        sd = (t[:, None, None] - spos).astype(jnp.float32)
        s = jnp.einsum('bgrqd,bgqnld->bgrqnl', qc, ks).astype(jnp.float32) \
            - slopes[None, :, :, None, None, None] * sd[:, :, None]
        smask = (sd >= 0)[:, :, None].reshape(B, G, 1, Q_BLOCK, m_sel)
        p = masked_softmax(s.reshape(B, G, R, Q_BLOCK, m_sel), smask)
        o_s = jnp.einsum('bgrqm,bgqmd->bgrqd', p.astype(vs.dtype), vs.reshape(B, G, Q_BLOCK, m_sel, hd))
        kw = lax.dynamic_slice_in_dim(kw_pad, ci * Q_BLOCK, Q_BLOCK + WINDOW, axis=2)
        vw = lax.dynamic_slice_in_dim(vw_pad, ci * Q_BLOCK, Q_BLOCK + WINDOW, axis=2)
        wpos = ci * Q_BLOCK - WINDOW + jnp.arange(Q_BLOCK + WINDOW)
        wd = t[:, None] - wpos[None, :]
        wmask = (wd >= 0) & (wd < WINDOW) & (wpos[None, :] >= 0)
        sw = jnp.einsum('bgrqd,bgmd->bgrqm', qc, kw).astype(jnp.float32) \
            - slopes[None, :, :, None, None] * wd.astype(jnp.float32)
        pw = masked_softmax(sw, wmask)
        o_w = jnp.einsum('bgrqm,bgmd->bgrqd', pw.astype(vw.dtype), vw)
        return o_s, o_w

    o_slc, o_win = lax.map(block_fn, (jnp.arange(n_q), q_chunks, idx_chunks))
    o_slc = o_slc.transpose(1, 2, 3, 0, 4, 5).reshape(B, G, R, S, hd)
    o_win = o_win.transpose(1, 2, 3, 0, 4, 5).reshape(B, G, R, S, hd)

    gates = jax.nn.sigmoid(gate_logits.astype(jnp.float32))
    gates = gates.reshape(B, S, G, R, N_NSA_BRANCH).transpose(0, 2, 3, 1, 4)
    o = gates[..., 0:1] * o_cmp + gates[..., 1:2] * o_slc + gates[..., 2:3] * o_win
    return o.transpose(0, 3, 1, 2, 4).reshape(B, S, NSA_WIDTH).astype(q.dtype)


def conv_mixer(u_glu, dw_w, dw_b, ln_g, ln_b):
    a, b = jnp.split(u_glu, 2, axis=-1)
    u = a * jax.nn.sigmoid(b)
    u = jnp.pad(u, ((0, 0), (CONV_TAPS - 1, 0), (0, 0)))
    y = lax.conv_general_dilated(u, dw_w[:, None, :], window_strides=(1,), padding='VALID',
                                 dimension_numbers=('NWC', 'WIO', 'NWC'),
                                 feature_group_count=CONV_WIDTH) + dw_b
    yf = y.astype(jnp.float32)
    mu = jnp.mean(yf, axis=-1, keepdims=True)
    var = jnp.mean(jnp.square(yf - mu), axis=-1, keepdims=True)
    yn = (yf - mu) * lax.rsqrt(var + NORM_EPS) * ln_g + ln_b
    return jax.nn.silu(yn).astype(u_glu.dtype)


def peer_ffn(h, w_q, sub_keys, u_tab, v_tab):
    B, S, D = h.shape
    T = B * S
    hf = h.reshape(T, D)
    q = (hf @ w_q).reshape(T, PEER_HEADS, 2, PEER_QDIM // 2)
    s = jnp.einsum('thcd,hcnd->thcn', q, sub_keys).astype(jnp.float32)
    s1, i1 = lax.top_k(s[:, :, 0], PEER_TOPK)
    s2, i2 = lax.top_k(s[:, :, 1], PEER_TOPK)
    cand = (s1[..., :, None] + s2[..., None, :]).reshape(T, PEER_HEADS, PEER_TOPK * PEER_TOPK)
    cand_idx = (i1[..., :, None] * PEER_NKEYS + i2[..., None, :]).reshape(T, PEER_HEADS, PEER_TOPK * PEER_TOPK)
    top_s, pos = lax.top_k(cand, PEER_TOPK)
    e_idx = jnp.take_along_axis(cand_idx, pos, axis=-1)
    gw = jax.nn.softmax(top_s, axis=-1).astype(h.dtype)
    n_blk = T // PEER_TOK_BLOCK

    def blk(args):
        hc, ec, gc = args
        ue = u_tab[ec]
        ve = v_tab[ec]
        act = jax.nn.gelu(jnp.einsum('td,thkd->thk', hc, ue), approximate=False)
        return jnp.einsum('thk,thkd->td', gc * act, ve)

    out = lax.map(blk, (hf.reshape(n_blk, PEER_TOK_BLOCK, D),
                        e_idx.reshape(n_blk, PEER_TOK_BLOCK, PEER_HEADS, PEER_TOPK),
                        gw.reshape(n_blk, PEER_TOK_BLOCK, PEER_HEADS, PEER_TOPK)))
    return out.reshape(B, S, D)


def setup_inputs(seed: int = 0) -> dict:
    key = jax.random.key(seed)
    ks = jax.random.split(key, 22)
    L = DEPTH

    def nrm(k, shape, scale):
        return jax.random.normal(k, shape, jnp.float32) * scale

    return {
        'x': nrm(ks[0], (BATCH, SEQ, D_MODEL), 1.0),
        'c': nrm(ks[1], (BATCH, D_MODEL), 1.0),
        'w_ada': nrm(ks[2], (L, D_MODEL, 6 * D_MODEL), 0.5 * D_MODEL ** -0.5),
        'b_ada': nrm(ks[3], (L, 6 * D_MODEL), 0.01),
        'norm_g': 1.0 + nrm(ks[4], (L, 2, D_MODEL), 0.02),
        'w_in': nrm(ks[5], (L, D_MODEL, IN_COLS), D_MODEL ** -0.5),
        'w_out': nrm(ks[6], (L, MIX_WIDTH, D_MODEL), MIX_WIDTH ** -0.5),
        'cmp_pe_k': nrm(ks[7], (L, CMP_LEN, HEAD_DIM), 0.1),
        'cmp_pe_v': nrm(ks[8], (L, CMP_LEN, HEAD_DIM), 0.1),
        'w_cmp_k': nrm(ks[9], (L, CMP_LEN, HEAD_DIM, HEAD_DIM), (CMP_LEN * HEAD_DIM) ** -0.5),
        'w_cmp_v': nrm(ks[10], (L, CMP_LEN, HEAD_DIM, HEAD_DIM), (CMP_LEN * HEAD_DIM) ** -0.5),
        'qk_norm_g': 1.0 + nrm(ks[11], (L, 4, HEAD_DIM), 0.02),
        'dw_w': nrm(ks[12], (L, CONV_TAPS, CONV_WIDTH), CONV_TAPS ** -0.5),
        'dw_b': nrm(ks[13], (L, CONV_WIDTH), 0.01),
        'conv_ln_g': 1.0 + nrm(ks[14], (L, CONV_WIDTH), 0.02),
        'conv_ln_b': nrm(ks[15], (L, CONV_WIDTH), 0.01),
        'peer_wq': nrm(ks[16], (L, D_MODEL, PEER_HEADS * PEER_QDIM), D_MODEL ** -0.5),
        'peer_sub_keys': nrm(ks[17], (L, PEER_HEADS, 2, PEER_NKEYS, PEER_QDIM // 2), (PEER_QDIM // 2) ** -0.5),
        'peer_u': nrm(ks[18], (L, PEER_EXPERTS, D_MODEL), D_MODEL ** -0.5),
        'peer_v': nrm(ks[19], (L, PEER_EXPERTS, D_MODEL), 0.5),
    }


def reference(x, c, w_ada, b_ada, norm_g, w_in, w_out, cmp_pe_k, cmp_pe_v, w_cmp_k, w_cmp_v,
              qk_norm_g, dw_w, dw_b, conv_ln_g, conv_ln_b, peer_wq, peer_sub_keys, peer_u, peer_v):
    B, S, _ = x.shape
    o_kv = N_Q_COLS
    o_gate = o_kv + N_KV_COLS
    o_glu = o_gate + N_GATE_COLS
    for l in range(DEPTH):
        mod = jax.nn.silu(c) @ w_ada[l] + b_ada[l]
        sh_m, sc_m, g_m, sh_f, sc_f, g_f = jnp.split(mod, 6, axis=-1)
        h = modulate(rmsnorm(x, norm_g[l, 0]), sh_m, sc_m)
        proj = h @ w_in[l]
        q = proj[..., :o_kv].reshape(B, S, NSA_HEADS, HEAD_DIM)
        kv = proj[..., o_kv:o_gate].reshape(B, S, 2 * N_NSA_BRANCH, NSA_KV_HEADS, HEAD_DIM)
        gl = proj[..., o_gate:o_glu].reshape(B, S, NSA_HEADS, N_NSA_BRANCH)
        glu = proj[..., o_glu:]
        a_out = nsa_mixer(q, kv, gl, cmp_pe_k[l], cmp_pe_v[l], w_cmp_k[l], w_cmp_v[l], qk_norm_g[l])
        c_out = conv_mixer(glu, dw_w[l], dw_b[l], conv_ln_g[l], conv_ln_b[l])
        mix = jnp.concatenate([a_out, c_out], axis=-1) @ w_out[l]
        x = x + g_m[:, None, :] * mix
        h = modulate(rmsnorm(x, norm_g[l, 1]), sh_f, sc_f)
        x = x + g_f[:, None, :] * peer_ffn(h, peer_wq[l], peer_sub_keys[l], peer_u[l], peer_v[l])
    return x
```

```python
import contextlib
import os
import numpy as np
import concourse.bass as bass
import concourse.mybir as mybir
from concourse.bass_utils import run_bass_kernel_spmd

F32 = mybir.dt.float32
BF16 = mybir.dt.bfloat16
U32 = mybir.dt.uint32
AF = mybir.ActivationFunctionType
ALU = mybir.AluOpType
AX = mybir.AxisListType

S = 2048
D = 1024
NT = 16
EPS = 1e-6
NEG = -30000.0
ENGS = ['pe', 'act', 'dve', 'pool', 'sp']


class Prog:
    def __init__(self, nc, stack):
        self.nc = nc
        self.stack = stack
        self.sems = {}
        self.eng_ops = {e: [] for e in ENGS}
        self.eng_cnt = {e: 0 for e in ENGS}
        self.dma_cnt = {}
        self.last_write = {}
        self.readers = {}
        self.waited = {e: {} for e in ENGS}

    def sem(self, name):
        if name not in self.sems:
            self.sems[name] = self.stack.enter_context(self.nc.semaphore(name))
        return self.sems[name]

    def _deps(self, eng, reads, writes):
        deps = {}

        def need(tok):
            if tok is None:
                return
            s, v = tok
            if deps.get(s, 0) < v:
                deps[s] = v
        for k in reads:
            need(self.last_write.get(k))
        for k in writes:
            need(self.last_write.get(k))
            for t in self.readers.get(k, ()):
                need(t)
        out = {}
        for s, v in deps.items():
            if s.startswith('D_'):
                v = max(v, 16 * self.dma_cnt.get(s, 0))
            if s == 'E_' + eng:
                if eng in ('pe', 'sp'):
                    continue
                if v < self.eng_cnt[eng] - 1:
                    continue
            if self.waited[eng].get(s, 0) >= v:
                continue
            self.waited[eng][s] = v
            out[s] = v
        return out

    def _commit(self, tok, reads, writes):
        for k in reads:
            self.readers.setdefault(k, []).append(tok)
        for k in writes:
            self.last_write[k] = tok
            self.readers[k] = []

    def op(self, eng, fn, reads=(), writes=()):
        deps = self._deps(eng, reads, writes)
        self.eng_cnt[eng] += 1
        tok = ('E_' + eng, self.eng_cnt[eng])
        self.sem(tok[0])
        self.eng_ops[eng].append((deps, fn, tok[0], 1))
        self._commit(tok, reads, writes)
        return tok

    def dma(self, eng, fn, sem, reads=(), writes=()):
        deps = self._deps(eng, reads, writes)
        s = 'D_' + sem
        self.dma_cnt[s] = self.dma_cnt.get(s, 0) + 1
        tok = (s, 16 * self.dma_cnt[s])
        self.sem(s)
        self.eng_ops[eng].append((deps, fn, s, 16))
        self._commit(tok, reads, writes)
        return tok

    def barrier(self):
        allv = {}
        for e in ENGS:
            if self.eng_cnt[e]:
                allv['E_' + e] = self.eng_cnt[e]
        for s, c in self.dma_cnt.items():
            allv[s] = 16 * c
        for e in ENGS:
            deps = {}
            for s, v in allv.items():
                if s == 'E_' + e:
                    continue
                if self.waited[e].get(s, 0) >= v:
                    continue
                self.waited[e][s] = v
                deps[s] = v
            self.eng_ops[e].append((deps, None, None, 0))

    def simulate(self):
        vals = getattr(self, '_simvals', {})
        ptr = {e: 0 for e in ENGS}
        ops = self.eng_ops
        progress = True
        while progress:
            progress = False
            for e in ENGS:
                while ptr[e] < len(ops[e]):
                    deps, fn, sname, inc = ops[e][ptr[e]]
                    if all(vals.get(s_, 0) >= v for s_, v in deps.items()):
                        if sname is not None:
                            vals[sname] = vals.get(sname, 0) + inc
                        ptr[e] += 1
                        progress = True
                    else:
                        break
        stuck = {e: (ptr[e], len(ops[e])) for e in ENGS if ptr[e] < len(ops[e])}
        if stuck:
            for e in stuck:
                deps = ops[e][ptr[e]][0]
                print("SIM STUCK", e, ptr[e], len(ops[e]), {s_: (v, vals.get(s_, 0)) for s_, v in deps.items() if vals.get(s_, 0) < v})
        else:
            print("SIM OK", {e: len(ops[e]) for e in ENGS})
        self._simvals = vals

    def flush(self):
        if os.environ.get('SIM'):
            self.simulate()
        nc = self.nc
        ops = self.eng_ops
        self.eng_ops = {e: [] for e in ENGS}
        sems = self.sems
        needed = {}
        for e in ENGS:
            for deps, fn, sname, inc in ops[e]:
                for s_, v in deps.items():
                    if s_.startswith('E_'):
                        needed.setdefault(s_, set()).add(v)
        if not hasattr(self, '_raw'):
            self._raw = {}
            self._new = {}
        newval = {}
        sig = {}
        for e in ENGS:
            s_ = 'E_' + e
            raw = self._raw.get(s_, 0)
            cur = self._new.get(s_, 0)
            nd = needed.get(s_, set())
            for idx, (deps, fn, sname, inc) in enumerate(ops[e]):
                if sname == s_:
                    raw += 1
                    if raw in nd:
                        cur += 1
                        newval[(s_, raw)] = cur
                        sig[(e, idx)] = True
            self._raw[s_] = raw
            self._new[s_] = cur
        for s_, nd in needed.items():
            for v in nd:
                assert (s_, v) in newval, ("wait on token from an earlier block", s_, v)

        with nc.Block() as block:
            def run(e, name):
                for idx, (deps, fn, sname, inc) in enumerate(ops[name]):
                    for s, v in deps.items():
                        if s.startswith('E_'):
                            v = newval[(s, v)]
                        e.wait_ge(sems[s], v)
                    if fn is not None:
                        inst = fn(e)
                        if inc == 16:
                            inst.then_inc(sems[sname], 16)
                        elif sig.get((name, idx)):
                            inst.then_inc(sems[sname], 1)

            @block.sync
            def _(e):
                run(e, 'sp')

            @block.scalar
            def _(e):
                run(e, 'act')

            @block.vector
            def _(e):
                run(e, 'dve')

            @block.gpsimd
            def _(e):
                run(e, 'pool')

            @block.tensor
            def _(e):
                run(e, 'pe')


def alibi_slopes():
    h = np.arange(1, 9, dtype=np.float32)
    return (2.0 ** (-8.0 * h / 8)).astype(np.float32)


def host_constants():
    sl = alibi_slopes()
    n = np.arange(128)
    c = {}
    t = np.arange(S)
    c['cmaskT'] = np.where(t[None, :] >= (16 * n[:, None] + 31), 0.0, NEG).astype(np.float32)
    tl = np.arange(128)
    tri = np.zeros((128, 2, 128), np.float32)
    tri[:, 0, :] = np.where(n[:, None] <= tl[None, :], 0.0, NEG)
    tri[:, 1, :] = np.where(n[:, None] > tl[None, :], 0.0, NEG)
    c['tri'] = tri
    E = np.zeros((32, 16, 128), np.float32)
    for kc in range(16):
        for nn in range(128):
            E[2 * kc + nn // 64, kc, nn] = 1.0
    c['esel'] = E
    dl = np.arange(16)
    c['biasw'] = (sl[None, :, None] * (-128.0 * dl[None, None, :] + n[:, None, None] - 64.0)).astype(np.float32)
    qt = np.arange(16)
    c['biasc'] = (sl[None, :, None] * (16.0 * n[:, None, None] + 31.0 - (qt[None, None, :] * 128.0 + 64.0))).astype(np.float32)
    ncmp = 127
    blk_tok0 = np.arange(ncmp) * 16
    sel_start = np.arange(32) * 64
    ov = np.clip(np.minimum(blk_tok0[:, None] + 32, sel_start[None, :] + 64)
                 - np.maximum(blk_tok0[:, None], sel_start[None, :]), 0, None).astype(np.float32) / 32.0
    ovl = np.zeros((128, 32), np.float32)
    ovl[:127] = ov
    c['ovl'] = ovl
    j = np.arange(32)
    validm = np.zeros((128, 8, 32), np.float32)
    selb = np.zeros((128, 8, 32), np.float32)
    for q in range(8, 16):
        tt = q * 128 + tl
        tb = tt // 64
        valid = j[None, :] <= tb[:, None]
        forced = (j[None, :] == 0) | (j[None, :] == tb[:, None]) | (j[None, :] == tb[:, None] - 1)
        validm[:, q - 8, :] = valid.astype(np.float32)
        selb[:, q - 8, :] = np.where(valid, 1.0e4 * forced, -1.0)
    c['validm'] = validm
    c['selb'] = selb
    c['iota16'] = np.tile(np.arange(16, dtype=np.float32)[None, :], (128, 1))
    return c


CONST_SHAPES = {
    'cmaskT': [128, 2048], 'tri': [128, 2, 128], 'esel': [32, 16, 128], 'biasw': [128, 8, 16],
    'biasc': [128, 8, 16], 'ovl': [128, 32], 'validm': [128, 8, 32], 'selb': [128, 8, 32], 'iota16': [128, 16],
}

IN_SHAPES = {
    'x': [S, D], 'cT': [128, 8], 'w_ada': [D, 6 * D], 'b_ada': [1, 6 * D], 'ng_bc': [128, 2, D],
    'w_in': [D, 2328], 'w_out': [D, D], 'pe_tab': [64, 4, 512], 'wck': [64, 32, 64], 'wcv': [64, 32, 64],
    'qkg_bc': [128, 4, 64], 'dwT': [128, 4, 31], 'cvec': [128, 3, 4], 'peer_wq': [D, 2048],
    'keysT': [128, 16, 128], 'peer_u': [16384, D], 'peer_v': [16384, D],
}


def build(stage=99, debug=False):
    nc = bass.Bass("TRN2", target_bir_lowering=False)
    dr = {}
    for k, shp in list(IN_SHAPES.items()) + list(CONST_SHAPES.items()):
        dr[k] = nc.dram_tensor(k, shp, F32, kind="ExternalInput").ap()
    out = nc.dram_tensor("out", [S, D], F32, kind="ExternalOutput").ap()
    x1s = nc.dram_tensor("x1s", [S, D], F32, kind=("ExternalOutput" if debug else "Internal")).ap()
    uvt = nc.dram_tensor("uvt", [16384, 2 * D], BF16, kind="Internal").ap()
    dbg = {}
    if debug:
        dbg['mod'] = nc.dram_tensor("dbg_mod", [128, 6 * D], F32, kind="ExternalOutput").ap()
        dbg['hT'] = nc.dram_tensor("dbg_hT", [128, 8, S], F32, kind="ExternalOutput").ap()
        dbg['cout'] = nc.dram_tensor("dbg_cout", [128, 4, S], F32, kind="ExternalOutput").ap()
        dbg['aout'] = nc.dram_tensor("dbg_aout", [S, 512], F32, kind="ExternalOutput").ap()
        dbg['kT'] = nc.dram_tensor("dbg_kT", [64, 4, S], F32, kind="ExternalOutput").ap()
        dbg['vaug'] = nc.dram_tensor("dbg_vaug", [128, 16 * 4 * 65], F32, kind="ExternalOutput").ap()
        dbg['kcnT'] = nc.dram_tensor("dbg_kcnT", [64, 256], F32, kind="ExternalOutput").ap()
        dbg['vcaug'] = nc.dram_tensor("dbg_vcaug", [128, 2 * 97], F32, kind="ExternalOutput").ap()

    with contextlib.ExitStack() as top:
        P = Prog(nc, top)

        def sbt(st, name, shape, dt):
            return st.enter_context(nc.sbuf_tensor("sb_" + name, shape, dt))

        psb = [top.enter_context(nc.psum_tensor("ps%d" % i, [128, 512], F32)) for i in range(8)]

        modbc = sbt(top, "modbc", [128, 6 * D], F32)
        ident = sbt(top, "ident", [128, 128], BF16)
        identf = sbt(top, "identf", [128, 128], F32)
        mhalf = sbt(top, "mhalf", [128, 16], F32)
        iota16 = sbt(top, "iota16", [128, 16], F32)

        P.op('pool', lambda e: e.memset(identf[:], 0.0), writes=['identf'])
        P.op('pool', lambda e: e.affine_select(out=identf[:], in_=identf[:], pattern=[[-1, 128]],
                                               compare_op=ALU.not_equal, fill=1.0, base=0, channel_multiplier=1),
             reads=['identf'], writes=['identf'])
        P.op('dve', lambda e: e.tensor_copy(out=ident[:], in_=identf[:]), reads=['identf'], writes=['ident'])
        P.op('pool', lambda e: e.memset(mhalf[:], -0.5), writes=['mhalf'])
        P.dma('sp', lambda e: e.dma_start(out=iota16[:], in_=dr['iota16']), 'c0', writes=['iota16'])

        def rsqrt_pool(out_ap, in_ap, n, rk, wk):
            P.op('pool', lambda e: e.tensor_tensor(out=out_ap, in0=in_ap, in1=mhalf[0:in_ap.shape[0], 0:n], op=ALU.pow),
                 reads=list(rk) + ['mhalf'], writes=wk)

        with contextlib.ExitStack() as mix:
            hT = sbt(mix, "hT", [128, 8, S], BF16)
            c_outT = sbt(mix, "c_outT", [128, 4, S], BF16)
            with contextlib.ExitStack() as pa:
                cT = sbt(pa, "cT", [128, 8], F32)
                csb = sbt(pa, "csb", [128, 8, 128], F32)
                brow = sbt(pa, "brow", [1, 6 * D], F32)
                onesr = sbt(pa, "onesr", [1, 128], F32)
                wa = [sbt(pa, "wa%d" % i, [128, 8, 512], F32) for i in range(2)]
                ngbc = sbt(pa, "ngbc", [128, 2, D], F32)
                xbuf = [sbt(pa, "xb%d" % i, [128, D], F32) for i in range(2)]
                junkb = sbt(pa, "junkb", [128, D], BF16)
                tmpf2 = [sbt(pa, "tmpf%d" % i, [128, D], F32) for i in range(2)]
                htok = [sbt(pa, "htok%d" % i, [128, D], BF16) for i in range(2)]
                ssA = sbt(pa, "ssA", [128, 16], F32)
                rvA = sbt(pa, "rvA", [128, 16], F32)
                rstdA = sbt(pa, "rstdA", [128, 16], F32)

                P.dma('sp', lambda e: e.dma_start(out=cT[:], in_=dr['cT']), 'c1', writes=['cT'])
                P.dma('sp', lambda e: e.dma_start(out=brow[:], in_=dr['b_ada']), 'c2', writes=['brow'])
                P.dma('sp', lambda e: e.dma_start(out=ngbc[:], in_=dr['ng_bc']), 'c3', writes=['ngbc'])
                P.op('pool', lambda e: e.memset(onesr[:], 1.0), writes=['onesr'])
                for j in range(8):
                    P.op('act', lambda e, j=j: e.activation(out=csb[:, j, :], in_=cT[:, j:j + 1].broadcast_to([128, 128]),
                                                            func=AF.Silu), reads=['cT'], writes=[('csb', j)])
                wada_v = dr['w_ada'].rearrange("(j p) c -> p j c", p=128)
                p1_next = [0]

                def pass1_upto(n):
                    while p1_next[0] < n:
                        tt = p1_next[0]
                        p1_next[0] += 1
                        b1 = tt % 2
                        xt1 = xbuf[b1]
                        P.dma('sp', lambda e, tt=tt, xt1=xt1: e.dma_start(out=xt1[:], in_=dr['x'][tt * 128:(tt + 1) * 128, :]),
                              'xa%d' % b1, writes=[('xa', b1)])
                        P.op('act', lambda e, tt=tt, xt1=xt1: e.activation(out=junkb[:], in_=xt1[:], func=AF.Square,
                                                                         accum_out=ssA[:, tt:tt + 1]),
                             reads=[('xa', b1)], writes=['junkb', ('ssA', tt)])
                for cb in range(12):
                    b = cb % 2
                    P.dma('sp', lambda e, cb=cb, b=b: e.dma_start(out=wa[b][:], in_=wada_v[:, :, cb * 512:(cb + 1) * 512]),
                          'wa%d' % b, writes=[('wa', b)])
                    pst = psb[cb % 2]
                    for j in range(8):
                        P.op('pe', lambda e, j=j, b=b, pst=pst: e.matmul(pst[:], lhsT=csb[:, j, :], rhs=wa[b][:, j, :],
                                                                         start=(j == 0), stop=False),
                             reads=[('csb', j), ('wa', b)], writes=[('ps', cb % 2)])
                    P.op('pe', lambda e, cb=cb, pst=pst: e.matmul(pst[:], lhsT=onesr[0:1, :], rhs=brow[0:1, cb * 512:(cb + 1) * 512],
                                                                  start=False, stop=True),
                         reads=['onesr', 'brow'], writes=[('ps', cb % 2)])
                    P.op('act', lambda e, cb=cb, pst=pst: e.activation(out=modbc[:, cb * 512:(cb + 1) * 512], in_=pst[:], func=AF.Copy),
                         reads=[('ps', cb % 2)], writes=[('mod', cb)])
                    pass1_upto(((cb + 1) * NT) // 12)
                pass1_upto(NT)
                ssk_all = [('ssA', t) for t in range(NT)]
                P.op('dve', lambda e: e.tensor_scalar(out=rvA[:], in0=ssA[:], scalar1=1.0 / D, scalar2=EPS, op0=ALU.mult, op1=ALU.add),
                     reads=ssk_all, writes=['rvA'])
                rsqrt_pool(rstdA[:], rvA[:], 16, ['rvA'], ['rstdA'])
                modkeys = [('mod', cb) for cb in range(12)]
                if debug:
                    P.dma('sp', lambda e: e.dma_start(out=dbg['mod'], in_=modbc[:]), 'dbg', reads=modkeys)
                P.op('dve', lambda e: e.scalar_tensor_tensor(out=modbc[:, D:2 * D], in0=modbc[:, D:2 * D], scalar=1.0,
                                                             in1=ngbc[:, 0, :], op0=ALU.add, op1=ALU.mult),
                     reads=modkeys + ['ngbc'], writes=['a_m'])
                P.op('dve', lambda e: e.scalar_tensor_tensor(out=modbc[:, 4 * D:5 * D], in0=modbc[:, 4 * D:5 * D], scalar=1.0,
                                                             in1=ngbc[:, 1, :], op0=ALU.add, op1=ALU.mult),
                     reads=modkeys + ['ngbc'], writes=['a_f'])
                sh_m = modbc[:, 0:D]
                a_m = modbc[:, D:2 * D]
                g_m = modbc[:, 2 * D:3 * D]
                sh_f = modbc[:, 3 * D:4 * D]
                a_f = modbc[:, 4 * D:5 * D]
                g_f = modbc[:, 5 * D:6 * D]

                for tt in range(NT):
                    b = tt % 2
                    xt = xbuf[b]
                    P.dma('sp', lambda e, tt=tt, xt=xt: e.dma_start(out=xt[:], in_=dr['x'][tt * 128:(tt + 1) * 128, :]),
                          'xa%d' % b, writes=[('xa', b)])
                    tf = tmpf2[b]
                    P.op('dve', lambda e, tt=tt, xt=xt, tf=tf: e.scalar_tensor_tensor(out=tf[:], in0=xt[:], scalar=rstdA[:, tt:tt + 1],
                                                                                      in1=a_m, op0=ALU.mult, op1=ALU.mult),
                         reads=[('xa', b), 'rstdA', 'a_m'], writes=[('tmpf', b)])
                    P.op('dve', lambda e, b=b, tf=tf: e.tensor_tensor(out=htok[b][:], in0=tf[:], in1=sh_m, op=ALU.add),
                         reads=[('tmpf', b)] + modkeys, writes=[('htok', b)])
                    pT = psb[2 + b][:].bitcast(BF16)
                    for j in range(8):
                        P.op('pe', lambda e, j=j, b=b, pT=pT: e.transpose(out=pT[:, j * 128:(j + 1) * 128],
                                                                          in_=htok[b][:, j * 128:(j + 1) * 128], identity=ident[:]),
                             reads=[('htok', b), 'ident'], writes=[('ps', 2 + b)])
                    P.op('act', lambda e, tt=tt, pT=pT: e.activation(out=hT[:, :, tt * 128:(tt + 1) * 128],
                                                                     in_=pT.rearrange("p (j t) -> p j t", j=8), func=AF.Copy),
                         reads=[('ps', 2 + b)], writes=[('hT', tt)])
                if debug:
                    hTf = sbt(pa, "hTf", [128, 8, S // 4], F32)
                    for q4 in range(4):
                        P.op('dve', lambda e, q4=q4: e.tensor_copy(out=hTf[:], in_=hT[:, :, q4 * 512:(q4 + 1) * 512]),
                             reads=[('hT', t) for t in range(NT)], writes=['hTf'])
                        P.dma('sp', lambda e, q4=q4: e.dma_start(out=dbg['hT'][:, :, q4 * 512:(q4 + 1) * 512], in_=hTf[:]), 'dbg', reads=['hTf'])
                P.barrier()
                P.flush()
            if stage <= 1:
                return nc
            hT_all = [('hT', t) for t in range(NT)]
            win_v = dr['w_in'].rearrange("(j p) c -> p j c", p=128)
            with contextlib.ExitStack() as pb:
                wg = sbt(pb, "wg", [128, 8, 1024], BF16)
                uT = sbt(pb, "uT", [128, 4, 30 + S], BF16)
                dwT = sbt(pb, "dwT", [128, 4, 31], F32)
                cvec = sbt(pb, "cvec", [128, 3, 4], F32)
                diag = sbt(pb, "diag", [128, 124, 128], BF16)
                sig = [sbt(pb, "sig%d" % i, [128, 512], F32) for i in range(2)]
                ysb = sbt(pb, "ysb", [128, 4, 512], F32)
                ysq = sbt(pb, "ysq", [128, 4, 512], F32)
                onesk = sbt(pb, "onesk", [128, 128], F32)
                msq = sbt(pb, "msq", [128, 512], F32)
                var = sbt(pb, "var", [128, 512], F32)
                rstdc = sbt(pb, "rstdc", [128, 512], F32)
                t1 = [sbt(pb, "t1_%d" % i, [128, 512], F32) for i in range(2)]
                for j in range(8):
                    P.dma('pool', lambda e, j=j: e.dma_start(out=wg[:, j, :], in_=win_v[:, j, 1304:2328]), 'wg', writes=['wg'])
                P.dma('sp', lambda e: e.dma_start(out=dwT[:], in_=dr['dwT']), 'c4', writes=['dwT'])
                P.dma('sp', lambda e: e.dma_start(out=cvec[:], in_=dr['cvec']), 'c5', writes=['cvec'])
                P.op('pool', lambda e: e.memset(onesk[:], 1.0 / 512.0), writes=['onesk'])
                P.op('pool', lambda e: e.memset(uT[:, :, 0:30], 0.0), writes=['uTpad'])
                for cc in range(4):
                    for k in range(31):
                        eng = 'dve'
                        P.op(eng, lambda e, cc=cc, k=k: e.tensor_scalar(out=diag[:, cc * 31 + k, :], in0=identf[:], scalar1=dwT[:, cc, k:k + 1],
                                                                        scalar2=None, op0=ALU.mult),
                             reads=['identf', 'dwT'], writes=[('diag', cc)])
                for tc in range(4):
                    tsl = slice(tc * 512, (tc + 1) * 512)
                    hkeys = [('hT', t) for t in range(tc * 4, tc * 4 + 4)]
                    for cc in range(4):
                        pa_ = psb[(2 * cc) % 4]
                        pb_ = psb[(2 * cc + 1) % 4]
                        ka, kb = ('ps', (2 * cc) % 4), ('ps', (2 * cc + 1) % 4)
                        for j in range(8):
                            P.op('pe', lambda e, j=j, cc=cc, pa_=pa_, tsl=tsl: e.matmul(pa_[:], lhsT=wg[:, j, cc * 128:(cc + 1) * 128], rhs=hT[:, j, tsl],
                                                                               start=(j == 0), stop=(j == 7)),
                                 reads=['wg'] + hkeys, writes=[ka])
                        for j in range(8):
                            P.op('pe', lambda e, j=j, cc=cc, pb_=pb_, tsl=tsl: e.matmul(pb_[:], lhsT=wg[:, j, 512 + cc * 128:512 + (cc + 1) * 128], rhs=hT[:, j, tsl],
                                                                               start=(j == 0), stop=(j == 7)),
                                 reads=['wg'] + hkeys, writes=[kb])
                        sg = sig[cc % 2]
                        P.op('act', lambda e, pb_=pb_, sg=sg: e.activation(out=sg[:], in_=pb_[:], func=AF.Sigmoid),
                             reads=[kb], writes=[('sig', cc % 2)])
                        P.op('dve', lambda e, cc=cc, pa_=pa_, sg=sg, tc=tc: e.tensor_tensor(out=uT[:, cc, 30 + tc * 512:30 + (tc + 1) * 512], in0=pa_[:], in1=sg[:], op=ALU.mult),
                             reads=[ka, ('sig', cc % 2)], writes=[('uT', cc, tc)])
                    for cc in range(4):
                        py = psb[4 + cc % 2]
                        ky = ('ps', 4 + cc % 2)
                        rd = [('uT', cc, tc), ('diag', cc), 'uTpad'] + ([('uT', cc, tc - 1)] if tc > 0 else [])
                        for k in range(31):
                            P.op('pe', lambda e, cc=cc, k=k, py=py, tc=tc: e.matmul(py[:], lhsT=diag[:, cc * 31 + k, :],
                                                                                  rhs=uT[:, cc, tc * 512 + k:tc * 512 + k + 512],
                                                                                  start=(k == 0), stop=(k == 30)),
                                 reads=rd, writes=[ky])
                        P.op('dve', lambda e, cc=cc, py=py: e.tensor_scalar(out=ysb[:, cc, :], in0=py[:], scalar1=cvec[:, 0, cc:cc + 1], scalar2=None, op0=ALU.add),
                             reads=[ky, 'cvec'], writes=[('ysb', cc)])
                        P.op('act', lambda e, cc=cc: e.activation(out=ysq[:, cc, :], in_=ysb[:, cc, :], func=AF.Square),
                             reads=[('ysb', cc)], writes=[('ysq', cc)])
                    pm, pe2 = psb[6], psb[7]
                    for cc in range(4):
                        P.op('pe', lambda e, cc=cc: e.matmul(pm[:], lhsT=onesk[:], rhs=ysb[:, cc, :], start=(cc == 0), stop=(cc == 3)),
                             reads=['onesk', ('ysb', cc)], writes=[('ps', 6)])
                    for cc in range(4):
                        P.op('pe', lambda e, cc=cc: e.matmul(pe2[:], lhsT=onesk[:], rhs=ysq[:, cc, :], start=(cc == 0), stop=(cc == 3)),
                             reads=['onesk', ('ysq', cc)], writes=[('ps', 7)])
                    P.op('act', lambda e: e.activation(out=msq[:], in_=pm[:], func=AF.Square), reads=[('ps', 6)], writes=['msq'])
                    P.op('dve', lambda e: e.tensor_tensor(out=var[:], in0=pe2[:], in1=msq[:], op=ALU.subtract), reads=[('ps', 7), 'msq'], writes=['var'])
                    P.op('dve', lambda e: e.tensor_scalar(out=var[:], in0=var[:], scalar1=0.0, scalar2=EPS, op0=ALU.max, op1=ALU.add), reads=['var'], writes=['var'])
                    P.op('act', lambda e: e.activation(out=var[:], in_=var[:], func=AF.Sqrt), reads=['var'], writes=['var'])
                    P.op('dve', lambda e: e.reciprocal(out=rstdc[:], in_=var[:]), reads=['var'], writes=['rstdc'])
                    for cc in range(4):
                        tb = t1[cc % 2]
                        P.op('dve', lambda e, cc=cc, tb=tb: e.tensor_tensor(out=tb[:], in0=ysb[:, cc, :], in1=pm[:], op=ALU.subtract),
                             reads=[('ysb', cc), ('ps', 6)], writes=[('t1', cc % 2)])
                        P.op('pool', lambda e, tb=tb: e.tensor_tensor(out=tb[:], in0=tb[:], in1=rstdc[:], op=ALU.mult),
                             reads=[('t1', cc % 2), 'rstdc'], writes=[('t1', cc % 2)])
                        P.op('act', lambda e, cc=cc, tb=tb, tsl=tsl: e.activation(out=c_outT[:, cc, tsl], in_=tb[:], func=AF.Silu,
                                                                         scale=cvec[:, 1, cc:cc + 1], bias=cvec[:, 2, cc:cc + 1]),
                             reads=[('t1', cc % 2), 'cvec'], writes=[('cout', cc, tc)])
                cout_all = [('cout', cc, tc) for cc in range(4) for tc in range(4)]
                if debug:
                    cof = sbt(pb, "cof", [128, 4, 512], F32)
                    for tc in range(4):
                        if os.environ.get('DBG_U'):
                            P.op('dve', lambda e, tc=tc: e.tensor_copy(out=cof[:], in_=uT[:, :, 30 + tc * 512:30 + (tc + 1) * 512]), reads=cout_all, writes=['cof'])
                        else:
                            P.op('dve', lambda e, tc=tc: e.tensor_copy(out=cof[:], in_=c_outT[:, :, tc * 512:(tc + 1) * 512]), reads=cout_all, writes=['cof'])
                        P.dma('sp', lambda e, tc=tc: e.dma_start(out=dbg['cout'][:, :, tc * 512:(tc + 1) * 512], in_=cof[:]), 'dbg', reads=['cof'])
                P.barrier()
                P.flush()
            if stage <= 2:
                return nc
            with contextlib.ExitStack() as pc:
                wkv = sbt(pc, "wkv", [128, 8, 768], BF16)
                wqg = sbt(pc, "wqg", [128, 8, 536], BF16)
                w_o = sbt(pc, "w_o", [128, 8, D], BF16)
                kT = sbt(pc, "kT", [64, 4, S], BF16)
                vaug = sbt(pc, "vaug", [128, 16, 4, 65], BF16)
                kAB = sbt(pc, "kAB", [64, 2, S], BF16)
                petab = sbt(pc, "petab", [64, 4, 512], F32)
                wc = sbt(pc, "wc", [64, 2, 32, 64], BF16)
                qkg = sbt(pc, "qkg", [128, 4, 64], F32)
                sqk = sbt(pc, "sqk", [128, 512], F32)
                ssk = sbt(pc, "ssk", [128, 8], F32)
                rstdk = sbt(pc, "rstdk", [128, 8], F32)
                tmpk = sbt(pc, "tmpk", [128, 512], F32)
                kn = sbt(pc, "kn", [128, 512], BF16)
                kcnT = sbt(pc, "kcnT", [64, 2, 128], BF16)
                vcaug = sbt(pc, "vcaug", [128, 2, 97], BF16)
                kcn = sbt(pc, "kcn", [128, 64], BF16)
                cmaskT = sbt(pc, "cmaskT", [128, S], BF16)
                tri = sbt(pc, "tri", [128, 2, 128], BF16)
                esel = sbt(pc, "esel", [32, 16, 128], BF16)
                biasw = sbt(pc, "biasw", [128, 8, 16], F32)
                biasc = sbt(pc, "biasc", [128, 8, 16], F32)
                validm = sbt(pc, "validm", [128, 8, 32], F32)
                selb = sbt(pc, "selb", [128, 8, 32], F32)
                small = sbt(pc, "small", [128, 64], F32)

                for j in range(8):
                    P.dma('pool', lambda e, j=j: e.dma_start(out=wkv[:, j, :], in_=win_v[:, j, 512:1280]), 'wkv', writes=['wkv'])
                for j in range(8):
                    P.dma('pool', lambda e, j=j: e.dma_start(out=wqg[:, j, 0:512], in_=win_v[:, j, 0:512]), 'wqg', writes=['wqg'])
                    P.dma('pool', lambda e, j=j: e.dma_start(out=wqg[:, j, 512:536], in_=win_v[:, j, 1280:1304]), 'wqg', writes=['wqg'])
                wout_v = dr['w_out'].rearrange("(j p) c -> p j c", p=128)
                for j in range(8):
                    P.dma('pool', lambda e, j=j: e.dma_start(out=w_o[:, j, :], in_=wout_v[:, j, :]), 'wo', writes=['w_o'])
                P.dma('pool', lambda e: e.dma_start(out=wc[:, 0, :, :], in_=dr['wck']), 'wc', writes=['wc'])
                P.dma('pool', lambda e: e.dma_start(out=wc[:, 1, :, :], in_=dr['wcv']), 'wc', writes=['wc'])
                P.dma('pool', lambda e: e.dma_start(out=cmaskT[:], in_=dr['cmaskT']), 'cst', writes=['cst'])
                P.dma('pool', lambda e: e.dma_start(out=tri[:], in_=dr['tri']), 'cst', writes=['cst'])
                P.dma('pool', lambda e: e.dma_start(out=esel[:], in_=dr['esel']), 'cst', writes=['cst'])
                P.op('pool', lambda e: e.memset(vcaug[:], 0.0), writes=['vcaug0'])
                P.op('pool', lambda e: e.memset(vcaug[:, :, 64:65], 1.0), reads=['vcaug0'], writes=['vcaug1'])
                for g in range(2):
                    P.dma('pool', lambda e, g=g: e.dma_start(out=vcaug[:, g, 65:97], in_=dr['ovl']), 'cst', reads=['vcaug0'], writes=['cst'])
                P.op('dve', lambda e: e.memset(vaug[:, :, :, 64:65], 1.0), writes=['vaug1'])
                P.dma('sp', lambda e: e.dma_start(out=petab[:], in_=dr['pe_tab']), 'c6', writes=['petab'])
                P.dma('sp', lambda e: e.dma_start(out=qkg[:], in_=dr['qkg_bc']), 'c6', writes=['qkg'])
                P.dma('sp', lambda e: e.dma_start(out=biasw[:], in_=dr['biasw']), 'c6', writes=['cst2'])
                P.dma('sp', lambda e: e.dma_start(out=biasc[:], in_=dr['biasc']), 'c6', writes=['cst2'])
                P.dma('sp', lambda e: e.dma_start(out=validm[:], in_=dr['validm']), 'c6', writes=['cst2'])
                P.dma('sp', lambda e: e.dma_start(out=selb[:], in_=dr['selb']), 'c6', writes=['cst2'])

                def rms_heads(ps_ap, nblk, key_in, wkey):
                    P.op('act', lambda e: e.activation(out=sqk[:, 0:nblk * 64], in_=ps_ap, func=AF.Square), reads=[key_in], writes=['sqk'])
                    P.op('dve', lambda e: e.tensor_reduce(out=ssk[:, 0:nblk], in_=sqk[:, 0:nblk * 64].rearrange("p (a b) -> p a b", b=64),
                                                          axis=AX.X, op=ALU.add), reads=['sqk'], writes=['ssk'])
                    P.op('dve', lambda e: e.tensor_scalar(out=ssk[:, 0:nblk], in0=ssk[:, 0:nblk], scalar1=1.0 / 64, scalar2=EPS,
                                                          op0=ALU.mult, op1=ALU.add), reads=['ssk'], writes=['ssk'])
                    rsqrt_pool(rstdk[:, 0:nblk], ssk[:, 0:nblk], nblk, ['ssk'], [wkey])

                ssk2 = sbt(pc, "ssk2", [128, 2, 8], F32)
                rstdk2 = sbt(pc, "rstdk2", [128, 2, 8], F32)

                def kv_stage_a(tt):
                    p = tt % 2
                    pk = psb[0] if p == 0 else psb[6]
                    pkk = ('ps', 0 if p == 0 else 6)
                    for j in range(8):
                        P.op('pe', lambda e, j=j: e.matmul(pk[:], lhsT=hT[:, j, tt * 128:(tt + 1) * 128], rhs=wkv[:, j, 256:768],
                                                           start=(j == 0), stop=(j == 7)),
                             reads=[('hT', tt), 'wkv'], writes=[pkk])
                    P.op('act', lambda e: e.activation(out=sqk[:], in_=pk[:], func=AF.Square), reads=[pkk], writes=['sqk'])
                    P.op('dve', lambda e: e.tensor_reduce(out=ssk2[:, p, :], in_=sqk[:].rearrange("p (a b) -> p a b", b=64), axis=AX.X, op=ALU.add),
                         reads=['sqk'], writes=[('ssk2', p)])
                    P.op('dve', lambda e: e.tensor_scalar(out=ssk2[:, p, :], in0=ssk2[:, p, :], scalar1=1.0 / 64, scalar2=EPS, op0=ALU.mult, op1=ALU.add),
                         reads=[('ssk2', p)], writes=[('ssk2', p)])
                    P.op('act', lambda e: e.activation(out=ssk2[:, p, :], in_=ssk2[:, p, :], func=AF.Sqrt), reads=[('ssk2', p)], writes=[('ssk2', p)])
                    P.op('dve', lambda e: e.reciprocal(out=rstdk2[:, p, :], in_=ssk2[:, p, :]), reads=[('ssk2', p)], writes=[('rstdk2', p)])
                    P.op('act', lambda e: e.activation(out=vaug[:, tt, 0:2, 0:64], in_=pk[:, 128:256].rearrange("p (a b) -> p a b", b=64), func=AF.Copy),
                         reads=[pkk, 'vaug1'], writes=[('vaug', tt)])
                    P.op('act', lambda e: e.activation(out=vaug[:, tt, 2:4, 0:64], in_=pk[:, 384:512].rearrange("p (a b) -> p a b", b=64), func=AF.Copy),
                         reads=[pkk, 'vaug1'], writes=[('vaug', tt)])

                def kv_stage_b(tt):
                    p = tt % 2
                    pk = psb[0] if p == 0 else psb[6]
                    pkk = ('ps', 0 if p == 0 else 6)
                    for bi, (blk, gi) in enumerate([(0, 2), (4, 3)]):
                        P.op('dve', lambda e, blk=blk, bi=bi: e.tensor_tensor(
                            out=tmpk[:, bi * 128:(bi + 1) * 128].rearrange("p (a b) -> p a b", b=64),
                            in0=pk[:, blk * 64:(blk + 2) * 64].rearrange("p (a b) -> p a b", b=64),
                            in1=rstdk2[:, p, blk:blk + 2].unsqueeze(2).broadcast_to([128, 2, 64]), op=ALU.mult),
                            reads=[pkk, ('rstdk2', p)], writes=[('tmpk', bi)])
                        P.op('dve', lambda e, bi=bi, gi=gi: e.tensor_tensor(
                            out=kn[:, bi * 128:(bi + 1) * 128].rearrange("p (a b) -> p a b", b=64),
                            in0=tmpk[:, bi * 128:(bi + 1) * 128].rearrange("p (a b) -> p a b", b=64),
                            in1=qkg[:, gi:gi + 1, :].broadcast_to([128, 2, 64]), op=ALU.mult),
                            reads=[('tmpk', bi), 'qkg'], writes=[('kn', bi)])
                    pT = psb[1][:].bitcast(BF16)
                    for i4 in range(4):
                        P.op('pe', lambda e, i4=i4: e.transpose(out=pT[0:64, i4 * 128:(i4 + 1) * 128], in_=kn[:, i4 * 64:(i4 + 1) * 64], identity=ident[:]),
                             reads=[('kn', i4 // 2), 'ident'], writes=[('ps', 1)])
                    P.op('act', lambda e: e.activation(out=kT[:, :, tt * 128:(tt + 1) * 128],
                                                       in_=pT[0:64, 0:512].rearrange("p (a b) -> p a b", b=128), func=AF.Copy),
                         reads=[('ps', 1)], writes=[('kT', tt)])

                kv_stage_a(0)
                for tt in range(NT):
                    if tt + 1 < NT:
                        kv_stage_a(tt + 1)
                    kv_stage_b(tt)
                for which in range(2):
                    for g in range(2):
                        for tc in range(4):
                            pf_ = psb[2 + tc % 2]
                            for j in range(8):
                                P.op('pe', lambda e, j=j, tc=tc, pf_=pf_, which=which, g=g: e.matmul(
                                    pf_[0:64, :], lhsT=wkv[:, j, which * 128 + g * 64:which * 128 + (g + 1) * 64], rhs=hT[:, j, tc * 512:(tc + 1) * 512],
                                    start=(j == 0), stop=(j == 7)), reads=hT_all + ['wkv'], writes=[('ps', 2 + tc % 2)])
                            for ab in range(2):
                                P.op('dve', lambda e, tc=tc, pf_=pf_, ab=ab, which=which: e.tensor_tensor(
                                    out=kAB[:, ab, tc * 512:(tc + 1) * 512], in0=pf_[0:64, :], in1=petab[:, which * 2 + ab, :], op=ALU.add),
                                    reads=[('ps', 2 + tc % 2), 'petab'], writes=[('kAB', tc)])
                        pcmp = psb[4]
                        for l in range(32):
                            ab = 0 if l < 16 else 1
                            P.op('pe', lambda e, l=l, ab=ab, which=which: e.matmul(
                                pcmp[0:127, 0:64], lhsT=kAB[:, ab, l:l + 16 * 126 + 1:16], rhs=wc[:, which, l, :], start=(l == 0), stop=(l == 31)),
                                reads=[('kAB', tc) for tc in range(4)] + ['wc'], writes=[('ps', 4)])
                        if which == 0:
                            P.op('act', lambda e: e.activation(out=sqk[0:127, 0:64], in_=pcmp[0:127, 0:64], func=AF.Square, accum_out=small[0:127, 0:1]),
                                 reads=[('ps', 4)], writes=['sqk', 'small'])
                            P.op('dve', lambda e: e.tensor_scalar(out=small[0:127, 1:2], in0=small[0:127, 0:1], scalar1=1.0 / 64, scalar2=EPS,
                                                                  op0=ALU.mult, op1=ALU.add), reads=['small'], writes=['small'])
                            rsqrt_pool(small[0:127, 2:3], small[0:127, 1:2], 1, ['small'], ['small'])
                            P.op('dve', lambda e: e.scalar_tensor_tensor(out=kcn[0:127, :], in0=pcmp[0:127, 0:64], scalar=small[0:127, 2:3],
                                                                         in1=qkg[0:127, 1, :], op0=ALU.mult, op1=ALU.mult),
                                 reads=[('ps', 4), 'small', 'qkg'], writes=['kcn'])
                            pT5 = psb[5][:].bitcast(BF16)
                            P.op('pe', lambda e: e.transpose(out=pT5[0:64, 0:127], in_=kcn[0:127, :], identity=ident[0:127, 0:127]),
                                 reads=['kcn', 'ident'], writes=[('ps', 5)])
                            P.op('act', lambda e, g=g: e.activation(out=kcnT[:, g, 0:127], in_=pT5[0:64, 0:127], func=AF.Copy),
                                 reads=[('ps', 5)], writes=[('kcnT', g)])
                        else:
                            P.op('act', lambda e, g=g: e.activation(out=vcaug[0:127, g, 0:64], in_=pcmp[0:127, 0:64], func=AF.Copy),
                                 reads=[('ps', 4), 'vcaug0'], writes=[('vcaug', g)])
                if debug and stage == 3:
                    kTf = sbt(pc, "kTf", [64, 4, 512], F32)
                    for q4 in range(4):
                        P.op('dve', lambda e, q4=q4: e.tensor_copy(out=kTf[:], in_=kT[:, :, q4 * 512:(q4 + 1) * 512]), reads=[('kT', t) for t in range(NT)], writes=['kTf'])
                        P.dma('sp', lambda e, q4=q4: e.dma_start(out=dbg['kT'][:, :, q4 * 512:(q4 + 1) * 512], in_=kTf[:]), 'dbg', reads=['kTf'])
                    vaf = sbt(pc, "vaf", [128, 16 * 4 * 65], F32)
                    P.op('dve', lambda e: e.tensor_copy(out=vaf[:], in_=vaug[:].rearrange("p a b c -> p (a b c)")), reads=[('vaug', t) for t in range(NT)] + ['vaug1'], writes=['vaf'])
                    P.dma('sp', lambda e: e.dma_start(out=dbg['vaug'], in_=vaf[:]), 'dbg', reads=['vaf'])
                    kcf = sbt(pc, "kcf", [64, 2 * 128], F32)
                    P.op('dve', lambda e: e.tensor_copy(out=kcf[:, 0:127], in_=kcnT[:, 0, 0:127]), reads=[('kcnT', 0)], writes=['kcf'])
                    P.op('dve', lambda e: e.tensor_copy(out=kcf[:, 128:255], in_=kcnT[:, 1, 0:127]), reads=[('kcnT', 1)], writes=['kcf'])
                    P.dma('sp', lambda e: e.dma_start(out=dbg['kcnT'], in_=kcf[:]), 'dbg', reads=['kcf'])
                    vcf = sbt(pc, "vcf", [128, 2 * 97], F32)
                    P.op('dve', lambda e: e.tensor_copy(out=vcf[:], in_=vcaug[:].rearrange("p a b -> p (a b)")), reads=[('vcaug', 0), ('vcaug', 1), 'vcaug1', 'cst'], writes=['vcf'])
                    P.dma('sp', lambda e: e.dma_start(out=dbg['vcaug'], in_=vcf[:]), 'dbg', reads=['vcf'])
                P.barrier()
                P.flush()
                if stage <= 3:
                    return nc

                qTt = sbt(pc, "qTt", [64, 8, 128], BF16)
                gates = sbt(pc, "gates", [128, 24], F32)
                qn = sbt(pc, "qn", [128, 512], BF16)
                pTs = [sbt(pc, "pT%d" % i, [128, 128], BF16) for i in range(8)]
                acc = sbt(pc, "acc", [128, 8, 64], F32)
                aout = sbt(pc, "aout", [128, 512], BF16)
                catT = sbt(pc, "catT", [128, 4, 128], BF16)
                impa = sbt(pc, "impa", [128, 32], F32)
                adj = sbt(pc, "adj", [128, 32], F32)
                adj2 = sbt(pc, "adj2", [128, 32], F32)
                top8 = sbt(pc, "top8", [128, 16], F32)
                negsel = sbt(pc, "negsel", [128, 32], BF16)
                negselT = sbt(pc, "negselT", [32, 128], BF16)
                dens = sbt(pc, "dens", [128, 8, 8], F32)
                xr = [sbt(pc, "xr%d" % i, [128, D], F32) for i in range(2)]
                x1t = [sbt(pc, "x1t%d" % i, [128, D], F32) for i in range(2)]
                if debug:
                    aof = sbt(pc, "aof", [128, 512], F32)
                kT_all = [('kT', t) for t in range(NT)]
                va_all = [('vaug', t) for t in range(NT)]

                qTt2 = [qTt, sbt(pc, "qTtB", [64, 8, 128], BF16)]
                gates2 = [gates, sbt(pc, "gatesB", [128, 24], F32)]

                def qprep(qt):
                    qsl = slice(qt * 128, (qt + 1) * 128)
                    qT_ = qTt2[qt % 2]
                    gt_ = gates2[qt % 2]
                    kq = ('qTt', qt % 2)
                    kg = ('gates', qt % 2)
                    pq = psb[0]
                    for j in range(8):
                        P.op('pe', lambda e, j=j: e.matmul(pq[:], lhsT=hT[:, j, qsl], rhs=wqg[:, j, 0:512], start=(j == 0), stop=(j == 7)),
                             reads=[('hT', qt), 'wqg'], writes=[('ps', 0)])
                    pg = psb[1]
                    for j in range(8):
                        P.op('pe', lambda e, j=j: e.matmul(pg[:, 0:24], lhsT=hT[:, j, qsl], rhs=wqg[:, j, 512:536], start=(j == 0), stop=(j == 7)),
                             reads=[('hT', qt), 'wqg'], writes=[('ps', 1)])
                    P.op('act', lambda e: e.activation(out=gt_[:], in_=pg[:, 0:24], func=AF.Exp, scale=-1.0), reads=[('ps', 1)], writes=[kg])
                    P.op('dve', lambda e: e.tensor_scalar(out=gt_[:], in0=gt_[:], scalar1=1.0, scalar2=None, op0=ALU.add), reads=[kg], writes=[kg])
                    P.op('dve', lambda e: e.reciprocal(out=gt_[:], in_=gt_[:]), reads=[kg], writes=[kg])
                    rms_heads(pq[:], 8, ('ps', 0), 'rstdq')
                    P.op('dve', lambda e: e.tensor_tensor(out=tmpk[:].rearrange("p (a b) -> p a b", b=64), in0=pq[:].rearrange("p (a b) -> p a b", b=64),
                                                          in1=rstdk[:, 0:8].unsqueeze(2).broadcast_to([128, 8, 64]), op=ALU.mult),
                         reads=[('ps', 0), 'rstdq'], writes=['tmpq'])
                    P.op('dve', lambda e: e.tensor_tensor(out=qn[:].rearrange("p (a b) -> p a b", b=64), in0=tmpk[:].rearrange("p (a b) -> p a b", b=64),
                                                          in1=qkg[:, 0:1, :].broadcast_to([128, 8, 64]), op=ALU.mult),
                         reads=['tmpq', 'qkg'], writes=['qn'])

                def qprep_b(qt):
                    qT_ = qTt2[qt % 2]
                    kq = ('qTt', qt % 2)
                    pT1 = psb[1][:].bitcast(BF16)
                    for h in range(8):
                        P.op('pe', lambda e, h=h: e.transpose(out=pT1[0:64, h * 128:(h + 1) * 128], in_=qn[:, h * 64:(h + 1) * 64], identity=ident[:]),
                             reads=['qn', 'ident'], writes=[('ps', 1)])
                    P.op('act', lambda e: e.activation(out=qT_[:], in_=pT1[0:64, :].rearrange("p (a b) -> p a b", b=128), func=AF.Copy),
                         reads=[('ps', 1)], writes=[kq])
                    P.dma('pool', lambda e, tt=qt: e.dma_start(out=uvt[tt * 1024:(tt + 1) * 1024, 0:D], in_=dr['peer_u'][tt * 1024:(tt + 1) * 1024, :]), 'uvt', writes=['uvt'])
                    P.dma('pool', lambda e, tt=qt: e.dma_start(out=uvt[tt * 1024:(tt + 1) * 1024, D:2 * D], in_=dr['peer_v'][tt * 1024:(tt + 1) * 1024, :]), 'uvt', writes=['uvt'])

                for qt in range(int(os.environ.get("NQT", NT))):
                    qsl = slice(qt * 128, (qt + 1) * 128)
                    if qt == 0:
                        qprep(0)
                        qprep_b(0)
                    qTt = qTt2[qt % 2]
                    gates = gates2[qt % 2]

                    for g in range(int(os.environ.get('NG', 2))):
                        jobs = []

                        def cmp_post(h, r, g=g, qt=qt):
                            gates = gates2[qt % 2]
                            kgt = ('gates', qt % 2)
                            ob = psb[2 + h % 2]
                            ok = ('ps', 2 + h % 2)
                            P.op('dve', lambda e: e.tensor_scalar(out=dens[:, h, 0:1], in0=ob[:, 64:65], scalar1=1e-30, scalar2=None, op0=ALU.max),
                                 reads=[ok], writes=[('dens', h)])
                            P.op('dve', lambda e: e.reciprocal(out=dens[:, h, 1:2], in_=dens[:, h, 0:1]), reads=[('dens', h)], writes=[('dens', h)])
                            P.op('dve', lambda e: e.tensor_tensor(out=dens[:, h, 2:3], in0=dens[:, h, 1:2], in1=gates[:, h * 3:h * 3 + 1], op=ALU.mult),
                                 reads=[('dens', h), kgt], writes=[('dens', h)])
                            P.op('dve', lambda e: e.tensor_scalar(out=acc[:, h, :], in0=ob[:, 0:64], scalar1=dens[:, h, 2:3], scalar2=None, op0=ALU.mult),
                                 reads=[ok, ('dens', h)], writes=[('acc', h)])
                            if qt >= 8:
                                if r == 0:
                                    P.op('dve', lambda e: e.tensor_scalar(out=impa[:], in0=ob[:, 65:97], scalar1=dens[:, h, 1:2], scalar2=None, op0=ALU.mult),
                                         reads=[ok, ('dens', h)], writes=['impa'])
                                else:
                                    P.op('dve', lambda e: e.scalar_tensor_tensor(out=impa[:], in0=ob[:, 65:97], scalar=dens[:, h, 1:2], in1=impa[:],
                                                                                 op0=ALU.mult, op1=ALU.add),
                                         reads=[ok, ('dens', h), 'impa'], writes=['impa'])
                                if r == 3:
                                    P.op('dve', lambda e: e.tensor_tensor(out=adj[:], in0=impa[:], in1=validm[:, qt - 8, :], op=ALU.mult),
                                         reads=['impa', 'cst2'], writes=['adj'])
                                    P.op('dve', lambda e: e.tensor_tensor(out=adj[:], in0=adj[:], in1=selb[:, qt - 8, :], op=ALU.add),
                                         reads=['adj', 'cst2'], writes=['adj'])
                                    P.op('dve', lambda e: e.max(out=top8[:, 0:8], in_=adj[:]), reads=['adj'], writes=['top8'])
                                    P.op('dve', lambda e: e.match_replace(out=adj2[:], in_to_replace=top8[:, 0:8], in_values=adj[:], imm_value=-1e30),
                                         reads=['adj', 'top8'], writes=['adj2'])
                                    P.op('dve', lambda e: e.max(out=top8[:, 8:16], in_=adj2[:]), reads=['adj2'], writes=['top8b'])
                                    P.op('dve', lambda e: e.tensor_scalar(out=negsel[:], in0=adj[:], scalar1=top8[:, 15:16], scalar2=NEG,
                                                                          op0=ALU.is_lt, op1=ALU.mult),
                                         reads=['adj', 'top8b'], writes=['negsel'])
                                    pT7 = psb[1][:].bitcast(BF16)
                                    P.op('pe', lambda e: e.transpose(out=pT7[0:32, 0:128], in_=negsel[:], identity=ident[:]),
                                         reads=['negsel', 'ident'], writes=[('ps', 1)])
                                    P.op('act', lambda e: e.activation(out=negselT[:], in_=pT7[0:32, 0:128], func=AF.Copy),
                                         reads=[('ps', 1)], writes=['negselT'])

                        def win_post(h, g=g, qt=qt):
                            gates = gates2[qt % 2]
                            kgt = ('gates', qt % 2)
                            ob = psb[2 + h % 2]
                            ok = ('ps', 2 + h % 2)
                            P.op('dve', lambda e: e.reciprocal(out=dens[:, h, 5:6], in_=ob[:, 320:321]), reads=[ok], writes=[('dens', h)])
                            P.op('dve', lambda e: e.tensor_tensor(out=dens[:, h, 7:8], in0=dens[:, h, 5:6], in1=gates[:, h * 3 + 2:h * 3 + 3], op=ALU.mult),
                                 reads=[('dens', h), kgt], writes=[('dens', h)])
                            P.op('dve', lambda e: e.scalar_tensor_tensor(out=acc[:, h, :], in0=ob[:, 256:320], scalar=dens[:, h, 7:8], in1=acc[:, h, :],
                                                                         op0=ALU.mult, op1=ALU.add),
                                 reads=[ok, ('dens', h), ('acc', h)], writes=[('acc', h)])

                        def fin_post(h, g=g, qt=qt):
                            gates = gates2[qt % 2]
                            kgt = ('gates', qt % 2)
                            ob = psb[2 + h % 2]
                            ok = ('ps', 2 + h % 2)
                            P.op('dve', lambda e: e.reciprocal(out=dens[:, h, 4:5], in_=ob[:, 192:193]), reads=[ok], writes=[('dens', h)])
                            P.op('dve', lambda e: e.tensor_tensor(out=dens[:, h, 6:7], in0=dens[:, h, 4:5], in1=gates[:, h * 3 + 1:h * 3 + 2], op=ALU.mult),
                                 reads=[('dens', h), kgt], writes=[('dens', h)])
                            P.op('dve', lambda e: e.scalar_tensor_tensor(out=aout[:, h * 64:(h + 1) * 64], in0=ob[:, 128:192], scalar=dens[:, h, 6:7],
                                                                         in1=acc[:, h, :], op0=ALU.mult, op1=ALU.add),
                                 reads=[ok, ('dens', h), ('acc', h)], writes=[('aout', h)])

                        for r in range(4):
                            h = g * 4 + r
                            jobs.append(dict(kind='cmp', h=h, r=r, nk=127, lhsT=kcnT[:, g, 0:127], lk=[('kcnT', g)],
                                             mask=('id', cmaskT[:, qsl]), bias=biasc[0:127, h, qt:qt + 1],
                                             v=vcaug[0:127, g, :], vk=[('vcaug', g), 'vcaug1', 'cst'], oc=(0, 97), obk=2 + h % 2, start=True, stop=True,
                                             post=(lambda h=h, r=r: cmp_post(h, r))))
                        win_jobs, slc_jobs = [], []
                        for r in range(4):
                            h = g * 4 + r
                            sl_h = float(alibi_slopes()[h])
                            skip_from = 99
                            for dl_ in range(1, 17):
                                if sl_h * (128.0 * (dl_ - 1) + 1.0) >= 60.0:
                                    skip_from = dl_
                                    break
                            wl = [kc for kc in range(max(0, qt - 4), qt + 1) if qt - kc < skip_from]
                            if os.environ.get('SKIPWIN'):
                                wl = []
                            for i, kc in enumerate(wl):
                                mask = None
                                if kc == qt:
                                    mask = ('id', tri[:, 0, :])
                                elif kc == qt - 4:
                                    mask = ('id', tri[:, 1, :])
                                win_jobs.append(dict(kind='win', h=h, r=r, nk=128, lhsT=kT[:, 2 + g, kc * 128:(kc + 1) * 128], lk=[('kT', kc)],
                                                 mask=mask, bias=biasw[:, h, qt - kc:qt - kc + 1], v=vaug[:, kc, 2 + g, :], vk=[('vaug', kc), 'vaug1'],
                                                 oc=(256, 321), obk=2 + h % 2, start=(i == 0), stop=(i == len(wl) - 1),
                                                 post=((lambda h=h: win_post(h)) if i == len(wl) - 1 else None)))
                            klist = [kc for kc in range(qt + 1) if qt - kc < skip_from]
                            for kc in klist:
                                mask = None
                                if kc == qt:
                                    mask = ('id', tri[:, 0, :])
                                elif qt >= 8:
                                    mask = ('sel', esel[:, kc, :])
                                gx = g + (2 if os.environ.get('E1') else 0)
                                slc_jobs.append(dict(kind='slc', h=h, r=r, nk=128, lhsT=kT[:, gx, kc * 128:(kc + 1) * 128], lk=[('kT', kc)],
                                                 mask=mask, bias=biasw[:, h, qt - kc:qt - kc + 1], v=vaug[:, kc, gx, :], vk=[('vaug', kc), 'vaug1'],
                                                 oc=(128, 193), obk=2 + h % 2, start=(kc == klist[0]), stop=(kc == qt),
                                                 post=((lambda h=h: fin_post(h)) if kc == qt else None)))
                        jobs = jobs + win_jobs + slc_jobs
                        LA = int(os.environ.get('LA', 3))
                        jobs = jobs[:int(os.environ.get('NJOBS', 100000))]
                        for i in range(len(jobs) + LA):
                            k2 = i - LA
                            if k2 >= 0 and not os.environ.get('NOPV'):
                                jb = jobs[k2]
                                sl_ = k2 % 8
                                sp_ = psb[4 + sl_ % 4][0:jb['nk'], 0:128]
                                skey = ('S', sl_ % 4)
                                pt_ = pTs[sl_]
                                P.op('act', lambda e, jb=jb, sp_=sp_, pt_=pt_: e.activation(out=pt_[0:jb['nk'], :], in_=sp_, func=AF.Exp,
                                                                                          bias=jb['bias'], scale=0.125),
                                     reads=[skey, 'cst2'], writes=[('pT', sl_)])
                                ob = psb[jb['obk']]
                                c0, c1 = jb['oc']
                                P.op('pe', lambda e, jb=jb, pt_=pt_, ob=ob, c0=c0, c1=c1: e.matmul(ob[:, c0:c1], lhsT=pt_[0:jb['nk'], :], rhs=jb['v'],
                                                                                                  start=jb['start'], stop=jb['stop']),
                                     reads=[('pT', sl_)] + jb['vk'], writes=[('ps', jb['obk'])])
                                if jb['post'] is not None and not os.environ.get('NOPOST'):
                                    jb['post']()
                            if i < len(jobs):
                                jb = jobs[i]
                                sl_ = i % 8
                                sp_ = psb[4 + sl_ % 4][0:jb['nk'], 0:128]
                                skey = ('S', sl_ % 4)
                                bkey = ('ps', 4 + sl_ // 4)
                                hm = jb['mask'] is not None and not os.environ.get('NOMASK')
                                P.op('pe', lambda e, jb=jb, sp_=sp_, hm=hm, qq=qTt2[qt % 2]: e.matmul(sp_, lhsT=jb['lhsT'], rhs=qq[:, jb['h'], :], start=True, stop=not hm),
                                     reads=jb['lk'] + [('qTt', qt % 2)], writes=[skey])
                                if g == 0 and i == len(jobs) // 2 and qt + 1 < NT:
                                    qprep(qt + 1)
                                if g == 1 and i == len(jobs) // 2 and qt + 1 < NT:
                                    qprep_b(qt + 1)
                                if hm:
                                    mk, mrhs = jb['mask']
                                    if mk == 'id':
                                        P.op('pe', lambda e, jb=jb, sp_=sp_, mrhs=mrhs: e.matmul(sp_, lhsT=ident[:, 0:jb['nk']], rhs=mrhs, start=False, stop=True),
                                             reads=['ident', 'cst'], writes=[skey])
                                    else:
                                        P.op('pe', lambda e, jb=jb, sp_=sp_, mrhs=mrhs: e.matmul(sp_, lhsT=mrhs, rhs=negselT[:], start=False, stop=True),
                                             reads=['negselT', 'cst'], writes=[skey])
                    aokeys = [('aout', h) for h in range(8)]
                    if debug:
                        P.op('dve', lambda e: e.tensor_copy(out=aof[:], in_=aout[:]), reads=aokeys, writes=['aof'])
                        P.dma('sp', lambda e, qsl=qsl: e.dma_start(out=dbg['aout'][qsl, :], in_=aof[:]), 'dbg2', reads=['aof'])
                    pT1 = psb[1][:].bitcast(BF16)
                    for j in range(4):
                        P.op('pe', lambda e, j=j: e.transpose(out=pT1[:, j * 128:(j + 1) * 128], in_=aout[:, j * 128:(j + 1) * 128], identity=ident[:]),
                             reads=aokeys + ['ident'], writes=[('ps', 1)])
                    P.op('act', lambda e: e.activation(out=catT[:], in_=pT1[:, 0:512].rearrange("p (a b) -> p a b", b=128), func=AF.Copy),
                         reads=[('ps', 1)], writes=['catT'])
                    b = qt % 2
                    P.dma('sp', lambda e, qsl=qsl, b=b: e.dma_start(out=xr[b][:], in_=dr['x'][qsl, :]), 'xr%d' % b, writes=[('xr', b)])
                    for half in range(2):
                        pm_ = psb[half]
                        hs = slice(half * 512, (half + 1) * 512)
                        for j in range(4):
                            P.op('pe', lambda e, j=j, pm_=pm_, hs=hs: e.matmul(pm_[:], lhsT=catT[:, j, :], rhs=w_o[:, j, hs], start=(j == 0), stop=False),
                                 reads=['catT', 'w_o'], writes=[('ps', half)])
                        for j in range(4):
                            P.op('pe', lambda e, j=j, pm_=pm_, hs=hs, qsl=qsl: e.matmul(pm_[:], lhsT=c_outT[:, j, qsl], rhs=w_o[:, 4 + j, hs], start=False, stop=(j == 3)),
                                 reads=['w_o'], writes=[('ps', half)])
                        P.op('dve', lambda e, pm_=pm_, hs=hs, b=b: e.tensor_tensor(out=x1t[b][:, hs], in0=pm_[:], in1=g_m[:, hs], op=ALU.mult),
                             reads=[('ps', half)], writes=[('x1t', b, half)])
                        P.op('dve', lambda e, hs=hs, b=b: e.tensor_tensor(out=x1t[b][:, hs], in0=x1t[b][:, hs], in1=xr[b][:, hs], op=ALU.add),
                             reads=[('x1t', b, half), ('xr', b)], writes=[('x1t', b, half)])
                    P.dma('sp', lambda e, qsl=qsl, b=b: e.dma_start(out=x1s[qsl, :], in_=x1t[b][:]), 'x1o%d' % b,
                          reads=[('x1t', b, 0), ('x1t', b, 1)], writes=[('x1s', qt)])
                P.barrier()
                P.flush()
            if stage <= 4:
                return nc
        with contextlib.ExitStack() as pp:
            wq = sbt(pp, "wq", [128, 8, 2048], BF16)
            keysT = sbt(pp, "keysT", [128, 16, 128], BF16)
            x1b = [sbt(pp, "x1b%d" % i, [128, D], F32) for i in range(3)]
            junk2 = sbt(pp, "junk2", [128, D], BF16)
            ss2 = sbt(pp, "ss2", [128, 4], F32)
            h2f = sbt(pp, "h2f", [128, D], F32)
            ep_tmp = sbt(pp, "ep_tmp", [128, D], F32)
            h2b = [sbt(pp, "h2b%d" % i, [128, D], BF16) for i in range(2)]
            h2T = sbt(pp, "h2T", [128, 8, 128], BF16)
            qpT = sbt(pp, "qpT", [128, 16, 128], BF16)
            big8 = sbt(pp, "big8", [128, 2048], F32)
            s_sb = big8[:].rearrange("p (a b) -> p a b", b=128)
            oh = big8[:].rearrange("p (a b c) -> p a b c", b=16, c=16)
            sw = sbt(pp, "sw", [128, 128], F32)
            v16 = sbt(pp, "v16", [128, 16, 16], F32)
            i16 = sbt(pp, "i16", [128, 16, 16], U32)
            i16f = sbt(pp, "i16f", [128, 16, 16], F32)
            cand = sbt(pp, "cand", [128, 8, 256], F32)
            cw = sbt(pp, "cw", [128, 256], F32)
            tv = sbt(pp, "tv", [128, 8, 16], F32)
            pos = sbt(pp, "pos", [128, 8, 16], U32)
            posf = sbt(pp, "posf", [128, 8, 16], F32)
            posa = sbt(pp, "posa", [128, 8, 16], U32)
            posb_ = sbt(pp, "posb_", [128, 8, 16], U32)
            af_ = sbt(pp, "af_", [128, 8, 16], F32)
            bf_ = sbt(pp, "bf_", [128, 8, 16], F32)
            thr16 = sbt(pp, "thr16", [128, 16], F32)
            i1 = sbt(pp, "i1", [128, 8, 16], F32)
            i2 = sbt(pp, "i2", [128, 8, 16], F32)
            ef = sbt(pp, "ef", [128, 128], F32)
            eidx = [sbt(pp, "eidx%d" % i, [128, 128], U32) for i in range(2)]
            gw = [sbt(pp, "gw%d" % i, [128, 8, 16], F32) for i in range(2)]
            esum = sbt(pp, "esum", [128, 8], F32)
            actv = sbt(pp, "actv", [128, 128], F32)
            coef = sbt(pp, "coef", [128, 128], F32)
            NBUF = int(os.environ.get('NBUF', 16))
            GS = int(os.environ.get('GS', 4))
            uvb = [sbt(pp, "uvb%d" % i, [128, 2 * D], BF16) for i in range(NBUF)]
            prodb = [sbt(pp, "prodb%d" % i, [128, D], BF16) for i in range(4)]
            Dk4 = [sbt(pp, "Dk4_%d" % i, [128, 4, 128], BF16) for i in range(3)]
            coefb = sbt(pp, "coefb", [128, 128], BF16)

            wq_v = dr['peer_wq'].rearrange("(j p) c -> p j c", p=128)
            for j in range(8):
                P.dma('pool', lambda e, j=j: e.dma_start(out=wq[:, j, :], in_=wq_v[:, j, :]), 'wq', writes=['wq'])
            P.dma('pool', lambda e: e.dma_start(out=keysT[:], in_=dr['keysT']), 'wq', writes=['keysT'])
            P.op('dve', lambda e: e.tensor_scalar(out=thr16[:], in0=iota16[:], scalar1=16.0, scalar2=16.0, op0=ALU.mult, op1=ALU.add),
                 reads=['iota16'], writes=['thr16'])

            def top16(T, src_ap, vout, iout, scratch, rk, wk):
                T(lambda: P.op('dve', lambda e: e.max(out=vout[:, 0:8], in_=src_ap), reads=rk, writes=[wk + 'v0']))
                T(lambda: P.op('dve', lambda e: e.max_index(out=iout[:, 0:8], in_max=vout[:, 0:8], in_values=src_ap), reads=rk + [wk + 'v0'], writes=[wk + 'i0']))
                T(lambda: P.op('dve', lambda e: e.match_replace(out=scratch, in_to_replace=vout[:, 0:8], in_values=src_ap, imm_value=-1e30),
                               reads=rk + [wk + 'v0'], writes=[wk + 'scr']))
                T(lambda: P.op('dve', lambda e: e.max(out=vout[:, 8:16], in_=scratch), reads=[wk + 'scr'], writes=[wk + 'v1']))
                T(lambda: P.op('dve', lambda e: e.max_index(out=iout[:, 8:16], in_max=vout[:, 8:16], in_values=scratch), reads=[wk + 'scr', wk + 'v1'], writes=[wk + 'i1']))
                return [wk + 'v0', wk + 'v1'], [wk + 'i0', wk + 'i1']

            def prologue(tt):
                th = []
                T = th.append
                b = tt % 2
                xb = tt % 3
                tsl = slice(tt * 128, (tt + 1) * 128)
                xt = x1b[xb]
                T(lambda: P.dma('sp', lambda e: e.dma_start(out=xt[:], in_=x1s[tsl, :]), 'x1i%d' % xb, reads=[('x1s', tt)], writes=[('x1b', xb)]))
                T(lambda: P.op('act', lambda e: e.activation(out=junk2[:], in_=xt[:], func=AF.Square, accum_out=ss2[:, 0:1]),
                               reads=[('x1b', xb)], writes=['junk2', 'ss2']))
                T(lambda: P.op('dve', lambda e: e.tensor_scalar(out=ss2[:, 1:2], in0=ss2[:, 0:1], scalar1=1.0 / D, scalar2=EPS, op0=ALU.mult, op1=ALU.add),
                               reads=['ss2'], writes=['ss2']))
                T(lambda: P.op('act', lambda e: e.activation(out=ss2[:, 2:3], in_=ss2[:, 1:2], func=AF.Sqrt), reads=['ss2'], writes=['ss2']))
                T(lambda: P.op('dve', lambda e: e.reciprocal(out=ss2[:, 3:4], in_=ss2[:, 2:3]), reads=['ss2'], writes=['ss2']))
                T(lambda: P.op('dve', lambda e: e.scalar_tensor_tensor(out=h2f[:], in0=xt[:], scalar=ss2[:, 3:4], in1=a_f, op0=ALU.mult, op1=ALU.mult),
                               reads=[('x1b', xb), 'ss2', 'a_f'], writes=['h2f']))
                T(lambda: P.op('dve', lambda e: e.tensor_tensor(out=h2b[b][:], in0=h2f[:], in1=sh_f, op=ALU.add), reads=['h2f'], writes=[('h2b', b)]))

                marks = {'pe_a': len(th)}

                def pe_a():
                    pT0 = psb[0][:].bitcast(BF16)
                    for j in range(8):
                        P.op('pe', lambda e, j=j: e.transpose(out=pT0[:, j * 128:(j + 1) * 128], in_=h2b[b][:, j * 128:(j + 1) * 128], identity=ident[:]),
                             reads=[('h2b', b), 'ident'], writes=[('ps', 0)])
                    P.op('act', lambda e: e.activation(out=h2T[:], in_=pT0.rearrange("p (a b) -> p a b", b=128), func=AF.Copy), reads=[('ps', 0)], writes=['h2T'])
                T(pe_a)
                marks['pe_b'] = len(th)

                def pe_b(q4, hc4):
                    pq_ = psb[(q4 + 1) % 2]
                    pk_ = ('ps', (q4 + 1) % 2)
                    hc = q4 * 4 + hc4
                    for j in range(8):
                        P.op('pe', lambda e, j=j: e.matmul(pq_[:, hc4 * 128:(hc4 + 1) * 128], lhsT=wq[:, j, hc * 128:(hc + 1) * 128],
                                                           rhs=h2T[:, j, :], start=(j == 0), stop=(j == 7)),
                             reads=['wq', 'h2T'], writes=[pk_])
                    if hc4 == 3:
                        P.op('act', lambda e: e.activation(out=qpT[:, q4 * 4:(q4 + 1) * 4, :], in_=pq_[:].rearrange("p (a b) -> p a b", b=128), func=AF.Copy),
                             reads=[pk_], writes=[('qpT', q4)])
                for q4 in range(4):
                    for hc4 in range(4):
                        T(lambda q4=q4, hc4=hc4: pe_b(q4, hc4))
                marks['pe_c'] = len(th)

                def pe_c(q4):
                    ps_ = psb[4 + q4 % 2]
                    for hc4 in range(4):
                        hc = q4 * 4 + hc4
                        P.op('pe', lambda e, hc=hc, hc4=hc4: e.matmul(ps_[:, hc4 * 128:(hc4 + 1) * 128], lhsT=qpT[:, hc, :], rhs=keysT[:, hc, :], start=True, stop=True),
                             reads=[('qpT', q4), 'keysT'], writes=[('ps', 4 + q4 % 2)])
                    P.op('act', lambda e: e.activation(out=s_sb[:, q4 * 4:(q4 + 1) * 4, :], in_=ps_[:].rearrange("p (a b) -> p a b", b=128), func=AF.Copy),
                         reads=[('ps', 4 + q4 % 2)], writes=['big8'])
                for q4 in range(4):
                    T(lambda q4=q4: pe_c(q4))
                marks['rest'] = len(th)
                vks, iks = [], []
                for hc in range(16):
                    a_, b_ = top16(T, s_sb[:, hc, :], v16[:, hc, :], i16[:, hc, :], sw[:], ['big8'], 't%d_' % hc)
                    vks += a_
                    iks += b_
                T(lambda: P.op('dve', lambda e: e.tensor_copy(out=i16f[:], in_=i16[:]), reads=iks, writes=['i16f']))
                v16v = v16[:].rearrange("p (h c) k -> p h c k", c=2)
                i16v = i16f[:].rearrange("p (h c) k -> p h c k", c=2)
                T(lambda: P.op('dve', lambda e: e.tensor_tensor(out=cand[:].rearrange("p h (a b) -> p h a b", b=16),
                                                                in0=v16v[:, :, 0, :].unsqueeze(3).broadcast_to([128, 8, 16, 16]),
                                                                in1=v16v[:, :, 1, :].unsqueeze(2).broadcast_to([128, 8, 16, 16]), op=ALU.add),
                               reads=vks, writes=['cand']))
                tks, pks = [], []
                for h in range(8):
                    a_, b_ = top16(T, cand[:, h, :], tv[:, h, :], pos[:, h, :], cw[:], ['cand'], 'c%d_' % h)
                    tks += a_
                    pks += b_
                T(lambda: P.op('dve', lambda e: e.tensor_scalar(out=posa[:], in0=pos[:], scalar1=4, scalar2=None, op0=ALU.logical_shift_right),
                               reads=pks, writes=['posa']))
                T(lambda: P.op('dve', lambda e: e.tensor_scalar(out=posb_[:], in0=pos[:], scalar1=15, scalar2=None, op0=ALU.bitwise_and),
                               reads=pks, writes=['posb_']))
                T(lambda: P.op('dve', lambda e: e.tensor_copy(out=af_[:], in_=posa[:]), reads=['posa'] + vks, writes=['af_']))
                T(lambda: P.op('dve', lambda e: e.tensor_copy(out=bf_[:], in_=posb_[:]), reads=['posb_'], writes=['bf_']))
                for (src, c_, dst, nm) in ((af_, 0, i1, 'i1'), (bf_, 1, i2, 'i2')):
                    T(lambda src=src: P.op('dve', lambda e: e.tensor_tensor(out=oh, in0=src[:].unsqueeze(3).broadcast_to([128, 8, 16, 16]),
                                                                            in1=iota16[:].unsqueeze(1).unsqueeze(1).broadcast_to([128, 8, 16, 16]), op=ALU.is_equal),
                                           reads=['af_', 'bf_', 'iota16'], writes=['big8']))
                    T(lambda c_=c_: P.op('dve', lambda e: e.tensor_tensor(out=oh, in0=oh, in1=i16v[:, :, c_, :].unsqueeze(2).broadcast_to([128, 8, 16, 16]), op=ALU.mult),
                                         reads=['big8', 'i16f'], writes=['big8']))
                    T(lambda dst=dst, nm=nm: P.op('dve', lambda e: e.tensor_reduce(out=dst[:], in_=oh, axis=AX.X, op=ALU.add), reads=['big8'], writes=[nm]))
                T(lambda: P.op('dve', lambda e: e.scalar_tensor_tensor(out=ef[:].rearrange("p (h k) -> p h k", k=16), in0=i1[:], scalar=128.0, in1=i2[:], op0=ALU.mult, op1=ALU.add),
                               reads=['i1', 'i2'], writes=['ef']))
                T(lambda: P.op('dve', lambda e: e.tensor_copy(out=eidx[b][:], in_=ef[:]), reads=['ef'], writes=[('eidx', b)]))
                gwb = gw[b]
                T(lambda: P.op('dve', lambda e: e.tensor_tensor(out=gwb[:], in0=tv[:], in1=tv[:, :, 0:1].broadcast_to([128, 8, 16]), op=ALU.subtract), reads=tks, writes=[('gw', b)]))
                T(lambda: P.op('act', lambda e: e.activation(out=gwb[:], in_=gwb[:], func=AF.Exp), reads=[('gw', b)], writes=[('gw', b)]))
                T(lambda: P.op('dve', lambda e: e.tensor_reduce(out=esum[:], in_=gwb[:], axis=AX.X, op=ALU.add), reads=[('gw', b)], writes=['esum']))
                T(lambda: P.op('dve', lambda e: e.reciprocal(out=esum[:], in_=esum[:]), reads=['esum'], writes=['esum']))
                T(lambda: P.op('dve', lambda e: e.tensor_tensor(out=gwb[:], in0=gwb[:], in1=esum[:].unsqueeze(2).broadcast_to([128, 8, 16]), op=ALU.mult),
                               reads=[('gw', b), 'esum'], writes=[('gw', b)]))
                return th, marks

            NPT = int(os.environ.get("NPT", NT))
            pend, _ = prologue(0)
            for f in pend:
                f()
            cnt = 0

            def make_sched(th, mk, ngrp):
                sc = {}
                f32 = ngrp / 32.0

                def put(g, f):
                    sc.setdefault(min(ngrp - 1, int(g * f32)), []).append(f)
                n_norm = mk['pe_a']
                norm_g = [0, 0, 1, 2, 3, 4, 4]
                for i in range(n_norm):
                    put(norm_g[i] if i < len(norm_g) else 4, th[i])
                put(6, th[mk['pe_a']])
                for i in range(mk['pe_b'], mk['pe_c']):
                    put(7 + (i - mk['pe_b']) // 8, th[i])
                for i in range(mk['pe_c'], mk['rest']):
                    put(10 + (i - mk['pe_c']) // 2, th[i])
                rest = th[mk['rest']:]
                g_lo, g_hi = 12, 31
                per_ = (len(rest) + (g_hi - g_lo) - 1) // (g_hi - g_lo)
                for i, f in enumerate(rest):
                    put(g_lo + i // per_, f)
                return sc

            def epilogue(tt):
                xb = tt % 3
                tsl = slice(tt * 128, (tt + 1) * 128)
                xt = x1b[xb]
                pob = 2 if tt % 2 == 0 else 6
                for half in range(2):
                    hs = slice(half * 512, (half + 1) * 512)
                    P.op('dve', lambda e, half=half, hs=hs: e.tensor_tensor(out=ep_tmp[:, hs], in0=psb[pob + half][:], in1=g_f[:, hs], op=ALU.mult),
                         reads=[('ps', pob + half)], writes=['ep_tmp'])
                    P.op('dve', lambda e, hs=hs: e.tensor_tensor(out=xt[:, hs], in0=ep_tmp[:, hs], in1=xt[:, hs], op=ALU.add),
                         reads=['ep_tmp', ('x1b', xb)], writes=[('x1b', xb)])
                P.dma('sp', lambda e: e.dma_start(out=out[tsl, :], in_=xt[:]), 'out%d' % xb, reads=[('x1b', xb)])

            dcnt = [0]
            assert GS <= 4
            ngrp = 128 // GS
            G0 = 2
            for tt in range(NPT):
                b = tt % 2
                if tt + 1 < NPT:
                    nth, nmk = prologue(tt + 1)
                    sched = make_sched(nth, nmk, ngrp)
                else:
                    sched = {}
                pob = 2 if tt % 2 == 0 else 6
                po = [psb[pob], psb[pob + 1]]
                slots = {}
                ti = 0
                for g in range(ngrp + 1):
                    if g < ngrp:
                        for k in range(GS):
                            hk = g * GS + k
                            sl_ = cnt % NBUF
                            slots[hk] = sl_
                            pr = prodb[cnt % 4]
                            cnt += 1
                            P.dma('pool', lambda e, hk=hk, sl_=sl_, b=b: e.indirect_dma_start(out=uvb[sl_][:], out_offset=None, in_=uvt,
                                                                                           in_offset=bass.IndirectOffsetOnAxis(ap=eidx[b][:, hk:hk + 1], axis=0)),
                                  'uv%d' % sl_, reads=[('eidx', b), 'uvt'], writes=[('uvb', sl_)])
                            P.op('dve', lambda e, sl_=sl_, pr=pr, b=b: e.tensor_tensor(out=pr[:], in0=uvb[sl_][:, 0:D], in1=h2b[b][:], op=ALU.mult),
                                 reads=[('uvb', sl_), ('h2b', b)], writes=[('prodb', id(pr))])
                            P.op('act', lambda e, hk=hk, pr=pr: e.activation(out=junk2[:], in_=pr[:], func=AF.Copy, accum_out=actv[:, hk:hk + 1]),
                                 reads=[('prodb', id(pr))], writes=['junk2', ('actv', g)])
                        gs = slice(g * GS, (g + 1) * GS)
                        P.op('act', lambda e, gs=gs: e.activation(out=coef[:, gs], in_=actv[:, gs], func=AF.Gelu), reads=[('actv', g)], writes=[('coef', g)])
                    if g >= 1:
                        g1 = g - 1
                        gs = slice(g1 * GS, (g1 + 1) * GS)
                        P.op('dve', lambda e, gs=gs, b=b: e.tensor_tensor(out=coefb[:, gs], in0=coef[:, gs], in1=gw[b][:].rearrange("p h k -> p (h k)")[:, gs], op=ALU.mult),
                             reads=[('coef', g1), ('gw', b)], writes=[('coefb', g1)])
                        dring = dcnt[0] % 3
                        dcnt[0] += 1
                        d4 = Dk4[dring]
                        P.op('dve', lambda e, gs=gs, d4=d4: e.tensor_tensor(out=d4[:, 0:GS, :], in0=ident[:].unsqueeze(1).broadcast_to([128, GS, 128]),
                                                                           in1=coefb[:, gs].unsqueeze(2).broadcast_to([128, GS, 128]), op=ALU.mult),
                             reads=['ident', ('coefb', g1)], writes=[('Dk4', dring)])
                        for k in range(GS):
                            hk = g1 * GS + k
                            sl_ = slots[hk]
                            for half in range(2):
                                P.op('pe', lambda e, hk=hk, d4=d4, k=k, sl_=sl_, half=half, po=po: e.matmul(po[half][:], lhsT=d4[:, k, :], rhs=uvb[sl_][:, D + half * 512:D + (half + 1) * 512],
                                                                                                           start=(hk == 0), stop=(hk == 127)),
                                     reads=[('Dk4', dring), ('uvb', sl_)], writes=[('ps', pob + half)])
                    if g == G0 and tt >= 1:
                        epilogue(tt - 1)
                    for f in sched.get(g, ()):
                        f()
            epilogue(NPT - 1)
            P.barrier()
            P.flush()
    return nc


def prep_inputs(inp):
    consts = host_constants()
    f = lambda a: np.ascontiguousarray(a, dtype=np.float32)
    shared = {}
    shared['w_ada'] = f(inp['w_ada'][0])
    shared['b_ada'] = f(inp['b_ada'][0][None, :])
    shared['ng_bc'] = f(np.broadcast_to(inp['norm_g'][0][None], (128, 2, D)))
    shared['w_in'] = f(inp['w_in'][0])
    shared['w_out'] = f(inp['w_out'][0])
    pk = inp['cmp_pe_k'][0]
    pv = inp['cmp_pe_v'][0]
    pe_tab = np.zeros((64, 4, 512), np.float32)
    for i, (p, lo) in enumerate([(pk, 0), (pk, 16), (pv, 0), (pv, 16)]):
        pe_tab[:, i, :] = np.tile(p[lo:lo + 16].T, (1, 32))
    shared['pe_tab'] = pe_tab
    shared['wck'] = f(inp['w_cmp_k'][0].transpose(1, 0, 2))
    shared['wcv'] = f(inp['w_cmp_v'][0].transpose(1, 0, 2))
    shared['qkg_bc'] = f(np.broadcast_to(inp['qk_norm_g'][0][None], (128, 4, 64)))
    shared['dwT'] = f(inp['dw_w'][0].reshape(31, 4, 128).transpose(2, 1, 0))
    cv = np.stack([inp['dw_b'][0], inp['conv_ln_g'][0], inp['conv_ln_b'][0]], 0)
    shared['cvec'] = f(cv.reshape(3, 4, 128).transpose(2, 0, 1))
    shared['peer_wq'] = f(inp['peer_wq'][0])
    shared['keysT'] = f(inp['peer_sub_keys'][0].reshape(16, 128, 128).transpose(2, 0, 1))
    shared['peer_u'] = f(inp['peer_u'][0])
    shared['peer_v'] = f(inp['peer_v'][0])
    shared.update(consts)
    maps = []
    for b in range(8):
        m = dict(shared)
        m['x'] = f(inp['x'][b])
        m['cT'] = f(inp['c'][b].reshape(8, 128).T)
        maps.append(m)
    return maps


def kernel(**inputs):
    maps = prep_inputs(inputs)
    nc = build()
    res = run_bass_kernel_spmd(nc, maps, core_ids=list(range(8)))
    return np.stack([r["out"] for r in res.results], axis=0).astype(np.float32)
```

```python
import contextlib
import os
import numpy as np
import concourse.bass as bass
import concourse.mybir as mybir
from concourse.bass_utils import run_bass_kernel_spmd

F32 = mybir.dt.float32
BF16 = mybir.dt.bfloat16
U32 = mybir.dt.uint32
AF = mybir.ActivationFunctionType
ALU = mybir.AluOpType
AX = mybir.AxisListType

S = 2048
D = 1024
NT = 16
EPS = 1e-6
NEG = -30000.0
ENGS = ['pe', 'act', 'dve', 'pool', 'sp']


class Prog:
    def __init__(self, nc, stack):
        self.nc = nc
        self.stack = stack
        self.sems = {}
        self.eng_ops = {e: [] for e in ENGS}
        self.eng_cnt = {e: 0 for e in ENGS}
        self.dma_cnt = {}
        self.last_write = {}
        self.readers = {}
        self.waited = {e: {} for e in ENGS}

    def sem(self, name):
        if name not in self.sems:
            self.sems[name] = self.stack.enter_context(self.nc.semaphore(name))
        return self.sems[name]

    def _deps(self, eng, reads, writes):
        deps = {}

        def need(tok):
            if tok is None:
                return
            s, v = tok
            if deps.get(s, 0) < v:
                deps[s] = v
        for k in reads:
            need(self.last_write.get(k))
        for k in writes:
            need(self.last_write.get(k))
            for t in self.readers.get(k, ()):
                need(t)
        out = {}
        for s, v in deps.items():
            if s.startswith('D_'):
                v = max(v, 16 * self.dma_cnt.get(s, 0))
            if s == 'E_' + eng:
                if eng in ('pe', 'sp'):
                    continue
                if v < self.eng_cnt[eng] - 1:
                    continue
            if self.waited[eng].get(s, 0) >= v:
                continue
            self.waited[eng][s] = v
            out[s] = v
        return out

    def _commit(self, tok, reads, writes):
        for k in reads:
            self.readers.setdefault(k, []).append(tok)
        for k in writes:
            self.last_write[k] = tok
            self.readers[k] = []

    def op(self, eng, fn, reads=(), writes=()):
        deps = self._deps(eng, reads, writes)
        self.eng_cnt[eng] += 1
        tok = ('E_' + eng, self.eng_cnt[eng])
        self.sem(tok[0])
        self.eng_ops[eng].append((deps, fn, tok[0], 1))
        self._commit(tok, reads, writes)
        return tok

    def dma(self, eng, fn, sem, reads=(), writes=()):
        deps = self._deps(eng, reads, writes)
        s = 'D_' + sem
        self.dma_cnt[s] = self.dma_cnt.get(s, 0) + 1
        tok = (s, 16 * self.dma_cnt[s])
        self.sem(s)
        self.eng_ops[eng].append((deps, fn, s, 16))
        self._commit(tok, reads, writes)
        return tok

    def barrier(self):
        allv = {}
        for e in ENGS:
            if self.eng_cnt[e]:
                allv['E_' + e] = self.eng_cnt[e]
        for s, c in self.dma_cnt.items():
            allv[s] = 16 * c
        for e in ENGS:
            deps = {}
            for s, v in allv.items():
                if s == 'E_' + e:
                    continue
                if self.waited[e].get(s, 0) >= v:
                    continue
                self.waited[e][s] = v
                deps[s] = v
            self.eng_ops[e].append((deps, None, None, 0))

    def simulate(self):
        vals = getattr(self, '_simvals', {})
        ptr = {e: 0 for e in ENGS}
        ops = self.eng_ops
        progress = True
        while progress:
            progress = False
            for e in ENGS:
                while ptr[e] < len(ops[e]):
                    deps, fn, sname, inc = ops[e][ptr[e]]
                    if all(vals.get(s_, 0) >= v for s_, v in deps.items()):
                        if sname is not None:
                            vals[sname] = vals.get(sname, 0) + inc
                        ptr[e] += 1
                        progress = True
                    else:
                        break
        stuck = {e: (ptr[e], len(ops[e])) for e in ENGS if ptr[e] < len(ops[e])}
        if stuck:
            for e in stuck:
                deps = ops[e][ptr[e]][0]
                print("SIM STUCK", e, ptr[e], len(ops[e]), {s_: (v, vals.get(s_, 0)) for s_, v in deps.items() if vals.get(s_, 0) < v})
        else:
            print("SIM OK", {e: len(ops[e]) for e in ENGS})
        self._simvals = vals

    def flush(self):
        if os.environ.get('SIM'):
            self.simulate()
        nc = self.nc
        ops = self.eng_ops
        self.eng_ops = {e: [] for e in ENGS}
        sems = self.sems
        needed = {}
        for e in ENGS:
            for deps, fn, sname, inc in ops[e]:
                for s_, v in deps.items():
                    if s_.startswith('E_'):
                        needed.setdefault(s_, set()).add(v)
        if not hasattr(self, '_raw'):
            self._raw = {}
            self._new = {}
        newval = {}
        sig = {}
        for e in ENGS:
            s_ = 'E_' + e
            raw = self._raw.get(s_, 0)
            cur = self._new.get(s_, 0)
            nd = needed.get(s_, set())
            for idx, (deps, fn, sname, inc) in enumerate(ops[e]):
                if sname == s_:
                    raw += 1
                    if raw in nd:
                        cur += 1
                        newval[(s_, raw)] = cur
                        sig[(e, idx)] = True
            self._raw[s_] = raw
            self._new[s_] = cur
        for s_, nd in needed.items():
            for v in nd:
                assert (s_, v) in newval, ("wait on token from an earlier block", s_, v)

        with nc.Block() as block:
            def run(e, name):
                for idx, (deps, fn, sname, inc) in enumerate(ops[name]):
                    for s, v in deps.items():
                        if s.startswith('E_'):
                            v = newval[(s, v)]
                        e.wait_ge(sems[s], v)
                    if fn is not None:
                        inst = fn(e)
                        if inc == 16:
                            inst.then_inc(sems[sname], 16)
                        elif sig.get((name, idx)):
                            inst.then_inc(sems[sname], 1)

            @block.sync
            def _(e):
                run(e, 'sp')

            @block.scalar
            def _(e):
                run(e, 'act')

            @block.vector
            def _(e):
                run(e, 'dve')

            @block.gpsimd
            def _(e):
                run(e, 'pool')

            @block.tensor
            def _(e):
                run(e, 'pe')


def alibi_slopes():
    h = np.arange(1, 9, dtype=np.float32)
    return (2.0 ** (-8.0 * h / 8)).astype(np.float32)


def host_constants():
    sl = alibi_slopes()
    n = np.arange(128)
    c = {}
    t = np.arange(S)
    c['cmaskT'] = np.where(t[None, :] >= (16 * n[:, None] + 31), 0.0, NEG).astype(np.float32)
    tl = np.arange(128)
    tri = np.zeros((128, 2, 128), np.float32)
    tri[:, 0, :] = np.where(n[:, None] <= tl[None, :], 0.0, NEG)
    tri[:, 1, :] = np.where(n[:, None] > tl[None, :], 0.0, NEG)
    c['tri'] = tri
    E = np.zeros((32, 16, 128), np.float32)
    for kc in range(16):
        for nn in range(128):
            E[2 * kc + nn // 64, kc, nn] = 1.0
    c['esel'] = E
    dl = np.arange(16)
    c['biasw'] = (sl[None, :, None] * (-128.0 * dl[None, None, :] + n[:, None, None] - 64.0)).astype(np.float32)
    qt = np.arange(16)
    c['biasc'] = (sl[None, :, None] * (16.0 * n[:, None, None] + 31.0 - (qt[None, None, :] * 128.0 + 64.0))).astype(np.float32)
    ncmp = 127
    blk_tok0 = np.arange(ncmp) * 16
    sel_start = np.arange(32) * 64
    ov = np.clip(np.minimum(blk_tok0[:, None] + 32, sel_start[None, :] + 64)
                 - np.maximum(blk_tok0[:, None], sel_start[None, :]), 0, None).astype(np.float32) / 32.0
    ovl = np.zeros((128, 32), np.float32)
    ovl[:127] = ov
    c['ovl'] = ovl
    j = np.arange(32)
    validm = np.zeros((128, 8, 32), np.float32)
    selb = np.zeros((128, 8, 32), np.float32)
    for q in range(8, 16):
        tt = q * 128 + tl
        tb = tt // 64
        valid = j[None, :] <= tb[:, None]
        forced = (j[None, :] == 0) | (j[None, :] == tb[:, None]) | (j[None, :] == tb[:, None] - 1)
        validm[:, q - 8, :] = valid.astype(np.float32)
        selb[:, q - 8, :] = np.where(valid, 1.0e4 * forced, -1.0)
    c['validm'] = validm
    c['selb'] = selb
    c['iota16'] = np.tile(np.arange(16, dtype=np.float32)[None, :], (128, 1))
    return c


CONST_SHAPES = {
    'cmaskT': [128, 2048], 'tri': [128, 2, 128], 'esel': [32, 16, 128], 'biasw': [128, 8, 16],
    'biasc': [128, 8, 16], 'ovl': [128, 32], 'validm': [128, 8, 32], 'selb': [128, 8, 32], 'iota16': [128, 16],
}

IN_SHAPES = {
    'x': [S, D], 'cT': [128, 8], 'w_ada': [D, 6 * D], 'b_ada': [1, 6 * D], 'ng_bc': [128, 2, D],
    'w_in': [D, 2328], 'w_out': [D, D], 'pe_tab': [64, 4, 512], 'wck': [64, 32, 64], 'wcv': [64, 32, 64],
    'qkg_bc': [128, 4, 64], 'dwT': [128, 4, 31], 'cvec': [128, 3, 4], 'peer_wq': [D, 2048],
    'keysT': [128, 16, 128], 'peer_u': [16384, D], 'peer_v': [16384, D],
}


def build(stage=99, debug=False):
    nc = bass.Bass("TRN2", target_bir_lowering=False)
    dr = {}
    for k, shp in list(IN_SHAPES.items()) + list(CONST_SHAPES.items()):
        dr[k] = nc.dram_tensor(k, shp, F32, kind="ExternalInput").ap()
    out = nc.dram_tensor("out", [S, D], F32, kind="ExternalOutput").ap()
    x1s = nc.dram_tensor("x1s", [S, D], F32, kind=("ExternalOutput" if debug else "Internal")).ap()
    uvt = nc.dram_tensor("uvt", [16384, 2 * D], BF16, kind="Internal").ap()
    dbg = {}
    if debug:
        dbg['mod'] = nc.dram_tensor("dbg_mod", [128, 6 * D], F32, kind="ExternalOutput").ap()
        dbg['hT'] = nc.dram_tensor("dbg_hT", [128, 8, S], F32, kind="ExternalOutput").ap()
        dbg['cout'] = nc.dram_tensor("dbg_cout", [128, 4, S], F32, kind="ExternalOutput").ap()
        dbg['aout'] = nc.dram_tensor("dbg_aout", [S, 512], F32, kind="ExternalOutput").ap()
        dbg['kT'] = nc.dram_tensor("dbg_kT", [64, 4, S], F32, kind="ExternalOutput").ap()
        dbg['vaug'] = nc.dram_tensor("dbg_vaug", [128, 16 * 4 * 65], F32, kind="ExternalOutput").ap()
        dbg['kcnT'] = nc.dram_tensor("dbg_kcnT", [64, 256], F32, kind="ExternalOutput").ap()
        dbg['vcaug'] = nc.dram_tensor("dbg_vcaug", [128, 2 * 97], F32, kind="ExternalOutput").ap()

    with contextlib.ExitStack() as top:
        P = Prog(nc, top)

        def sbt(st, name, shape, dt):
            return st.enter_context(nc.sbuf_tensor("sb_" + name, shape, dt))

        psb = [top.enter_context(nc.psum_tensor("ps%d" % i, [128, 512], F32)) for i in range(8)]

        modbc = sbt(top, "modbc", [128, 6 * D], F32)
        ident = sbt(top, "ident", [128, 128], BF16)
        identf = sbt(top, "identf", [128, 128], F32)
        mhalf = sbt(top, "mhalf", [128, 16], F32)
        iota16 = sbt(top, "iota16", [128, 16], F32)

        P.op('pool', lambda e: e.memset(identf[:], 0.0), writes=['identf'])
        P.op('pool', lambda e: e.affine_select(out=identf[:], in_=identf[:], pattern=[[-1, 128]],
                                               compare_op=ALU.not_equal, fill=1.0, base=0, channel_multiplier=1),
             reads=['identf'], writes=['identf'])
        P.op('dve', lambda e: e.tensor_copy(out=ident[:], in_=identf[:]), reads=['identf'], writes=['ident'])
        P.op('pool', lambda e: e.memset(mhalf[:], -0.5), writes=['mhalf'])
        P.dma('sp', lambda e: e.dma_start(out=iota16[:], in_=dr['iota16']), 'c0', writes=['iota16'])

        def rsqrt_pool(out_ap, in_ap, n, rk, wk):
            P.op('pool', lambda e: e.tensor_tensor(out=out_ap, in0=in_ap, in1=mhalf[0:in_ap.shape[0], 0:n], op=ALU.pow),
                 reads=list(rk) + ['mhalf'], writes=wk)

        with contextlib.ExitStack() as mix:
            hT = sbt(mix, "hT", [128, 8, S], BF16)
            c_outT = sbt(mix, "c_outT", [128, 4, S], BF16)
            with contextlib.ExitStack() as pa:
                cT = sbt(pa, "cT", [128, 8], F32)
                csb = sbt(pa, "csb", [128, 8, 128], F32)
                brow = sbt(pa, "brow", [1, 6 * D], F32)
                onesr = sbt(pa, "onesr", [1, 128], F32)
                wa = [sbt(pa, "wa%d" % i, [128, 8, 512], F32) for i in range(2)]
                ngbc = sbt(pa, "ngbc", [128, 2, D], F32)
                xbuf = [sbt(pa, "xb%d" % i, [128, D], F32) for i in range(2)]
                junkb = sbt(pa, "junkb", [128, D], BF16)
                tmpf2 = [sbt(pa, "tmpf%d" % i, [128, D], F32) for i in range(2)]
                htok = [sbt(pa, "htok%d" % i, [128, D], BF16) for i in range(2)]
                ssA = sbt(pa, "ssA", [128, 16], F32)
                rvA = sbt(pa, "rvA", [128, 16], F32)
                rstdA = sbt(pa, "rstdA", [128, 16], F32)

                P.dma('sp', lambda e: e.dma_start(out=cT[:], in_=dr['cT']), 'c1', writes=['cT'])
                P.dma('sp', lambda e: e.dma_start(out=brow[:], in_=dr['b_ada']), 'c2', writes=['brow'])
                P.dma('sp', lambda e: e.dma_start(out=ngbc[:], in_=dr['ng_bc']), 'c3', writes=['ngbc'])
                P.op('pool', lambda e: e.memset(onesr[:], 1.0), writes=['onesr'])
                for j in range(8):
                    P.op('act', lambda e, j=j: e.activation(out=csb[:, j, :], in_=cT[:, j:j + 1].broadcast_to([128, 128]),
                                                            func=AF.Silu), reads=['cT'], writes=[('csb', j)])
                wada_v = dr['w_ada'].rearrange("(j p) c -> p j c", p=128)
                p1_next = [0]

                def pass1_upto(n):
                    while p1_next[0] < n:
                        tt = p1_next[0]
                        p1_next[0] += 1
                        b1 = tt % 2
                        xt1 = xbuf[b1]
                        P.dma('sp', lambda e, tt=tt, xt1=xt1: e.dma_start(out=xt1[:], in_=dr['x'][tt * 128:(tt + 1) * 128, :]),
                              'xa%d' % b1, writes=[('xa', b1)])
                        P.op('act', lambda e, tt=tt, xt1=xt1: e.activation(out=junkb[:], in_=xt1[:], func=AF.Square,
                                                                         accum_out=ssA[:, tt:tt + 1]),
                             reads=[('xa', b1)], writes=['junkb', ('ssA', tt)])
                for cb in range(12):
                    b = cb % 2
                    P.dma('sp', lambda e, cb=cb, b=b: e.dma_start(out=wa[b][:], in_=wada_v[:, :, cb * 512:(cb + 1) * 512]),
                          'wa%d' % b, writes=[('wa', b)])
                    pst = psb[cb % 2]
                    for j in range(8):
                        P.op('pe', lambda e, j=j, b=b, pst=pst: e.matmul(pst[:], lhsT=csb[:, j, :], rhs=wa[b][:, j, :],
                                                                         start=(j == 0), stop=False),
                             reads=[('csb', j), ('wa', b)], writes=[('ps', cb % 2)])
                    P.op('pe', lambda e, cb=cb, pst=pst: e.matmul(pst[:], lhsT=onesr[0:1, :], rhs=brow[0:1, cb * 512:(cb + 1) * 512],
                                                                  start=False, stop=True),
                         reads=['onesr', 'brow'], writes=[('ps', cb % 2)])
                    P.op('act', lambda e, cb=cb, pst=pst: e.activation(out=modbc[:, cb * 512:(cb + 1) * 512], in_=pst[:], func=AF.Copy),
                         reads=[('ps', cb % 2)], writes=[('mod', cb)])
                    pass1_upto(((cb + 1) * NT) // 12)
                pass1_upto(NT)
                ssk_all = [('ssA', t) for t in range(NT)]
                P.op('dve', lambda e: e.tensor_scalar(out=rvA[:], in0=ssA[:], scalar1=1.0 / D, scalar2=EPS, op0=ALU.mult, op1=ALU.add),
                     reads=ssk_all, writes=['rvA'])
                rsqrt_pool(rstdA[:], rvA[:], 16, ['rvA'], ['rstdA'])
                modkeys = [('mod', cb) for cb in range(12)]
                if debug:
                    P.dma('sp', lambda e: e.dma_start(out=dbg['mod'], in_=modbc[:]), 'dbg', reads=modkeys)
                P.op('dve', lambda e: e.scalar_tensor_tensor(out=modbc[:, D:2 * D], in0=modbc[:, D:2 * D], scalar=1.0,
                                                             in1=ngbc[:, 0, :], op0=ALU.add, op1=ALU.mult),
                     reads=modkeys + ['ngbc'], writes=['a_m'])
                P.op('dve', lambda e: e.scalar_tensor_tensor(out=modbc[:, 4 * D:5 * D], in0=modbc[:, 4 * D:5 * D], scalar=1.0,
                                                             in1=ngbc[:, 1, :], op0=ALU.add, op1=ALU.mult),
                     reads=modkeys + ['ngbc'], writes=['a_f'])
                sh_m = modbc[:, 0:D]
                a_m = modbc[:, D:2 * D]
                g_m = modbc[:, 2 * D:3 * D]
                sh_f = modbc[:, 3 * D:4 * D]
                a_f = modbc[:, 4 * D:5 * D]
                g_f = modbc[:, 5 * D:6 * D]

                for tt in range(NT):
                    b = tt % 2
                    xt = xbuf[b]
                    P.dma('sp', lambda e, tt=tt, xt=xt: e.dma_start(out=xt[:], in_=dr['x'][tt * 128:(tt + 1) * 128, :]),
                          'xa%d' % b, writes=[('xa', b)])
                    tf = tmpf2[b]
                    P.op('dve', lambda e, tt=tt, xt=xt, tf=tf: e.scalar_tensor_tensor(out=tf[:], in0=xt[:], scalar=rstdA[:, tt:tt + 1],
                                                                                      in1=a_m, op0=ALU.mult, op1=ALU.mult),
                         reads=[('xa', b), 'rstdA', 'a_m'], writes=[('tmpf', b)])
                    P.op('dve', lambda e, b=b, tf=tf: e.tensor_tensor(out=htok[b][:], in0=tf[:], in1=sh_m, op=ALU.add),
                         reads=[('tmpf', b)] + modkeys, writes=[('htok', b)])
                    pT = psb[2 + b][:].bitcast(BF16)
                    for j in range(8):
                        P.op('pe', lambda e, j=j, b=b, pT=pT: e.transpose(out=pT[:, j * 128:(j + 1) * 128],
                                                                          in_=htok[b][:, j * 128:(j + 1) * 128], identity=ident[:]),
                             reads=[('htok', b), 'ident'], writes=[('ps', 2 + b)])
                    P.op('act', lambda e, tt=tt, pT=pT: e.activation(out=hT[:, :, tt * 128:(tt + 1) * 128],
                                                                     in_=pT.rearrange("p (j t) -> p j t", j=8), func=AF.Copy),
                         reads=[('ps', 2 + b)], writes=[('hT', tt)])
                if debug:
                    hTf = sbt(pa, "hTf", [128, 8, S // 4], F32)
                    for q4 in range(4):
                        P.op('dve', lambda e, q4=q4: e.tensor_copy(out=hTf[:], in_=hT[:, :, q4 * 512:(q4 + 1) * 512]),
                             reads=[('hT', t) for t in range(NT)], writes=['hTf'])
                        P.dma('sp', lambda e, q4=q4: e.dma_start(out=dbg['hT'][:, :, q4 * 512:(q4 + 1) * 512], in_=hTf[:]), 'dbg', reads=['hTf'])
                P.barrier()
                P.flush()
            if stage <= 1:
                return nc
            hT_all = [('hT', t) for t in range(NT)]
            win_v = dr['w_in'].rearrange("(j p) c -> p j c", p=128)
            with contextlib.ExitStack() as pb:
                wg = sbt(pb, "wg", [128, 8, 1024], BF16)
                uT = sbt(pb, "uT", [128, 4, 30 + S], BF16)
                dwT = sbt(pb, "dwT", [128, 4, 31], F32)
                cvec = sbt(pb, "cvec", [128, 3, 4], F32)
                diag = sbt(pb, "diag", [128, 124, 128], BF16)
                sig = [sbt(pb, "sig%d" % i, [128, 512], F32) for i in range(2)]
                ysb = sbt(pb, "ysb", [128, 4, 512], F32)
                ysq = sbt(pb, "ysq", [128, 4, 512], F32)
                onesk = sbt(pb, "onesk", [128, 128], F32)
                msq = sbt(pb, "msq", [128, 512], F32)
                var = sbt(pb, "var", [128, 512], F32)
                rstdc = sbt(pb, "rstdc", [128, 512], F32)
                t1 = [sbt(pb, "t1_%d" % i, [128, 512], F32) for i in range(2)]
                for j in range(8):
                    P.dma('pool', lambda e, j=j: e.dma_start(out=wg[:, j, :], in_=win_v[:, j, 1304:2328]), 'wg', writes=['wg'])
                P.dma('sp', lambda e: e.dma_start(out=dwT[:], in_=dr['dwT']), 'c4', writes=['dwT'])
                P.dma('sp', lambda e: e.dma_start(out=cvec[:], in_=dr['cvec']), 'c5', writes=['cvec'])
                P.op('pool', lambda e: e.memset(onesk[:], 1.0 / 512.0), writes=['onesk'])
                P.op('pool', lambda e: e.memset(uT[:, :, 0:30], 0.0), writes=['uTpad'])
                for cc in range(4):
                    for k in range(31):
                        eng = 'dve'
                        P.op(eng, lambda e, cc=cc, k=k: e.tensor_scalar(out=diag[:, cc * 31 + k, :], in0=identf[:], scalar1=dwT[:, cc, k:k + 1],
                                                                        scalar2=None, op0=ALU.mult),
                             reads=['identf', 'dwT'], writes=[('diag', cc)])
                for tc in range(4):
                    tsl = slice(tc * 512, (tc + 1) * 512)
                    hkeys = [('hT', t) for t in range(tc * 4, tc * 4 + 4)]
                    for cc in range(4):
                        pa_ = psb[(2 * cc) % 4]
                        pb_ = psb[(2 * cc + 1) % 4]
                        ka, kb = ('ps', (2 * cc) % 4), ('ps', (2 * cc + 1) % 4)
                        for j in range(8):
                            P.op('pe', lambda e, j=j, cc=cc, pa_=pa_, tsl=tsl: e.matmul(pa_[:], lhsT=wg[:, j, cc * 128:(cc + 1) * 128], rhs=hT[:, j, tsl],
                                                                               start=(j == 0), stop=(j == 7)),
                                 reads=['wg'] + hkeys, writes=[ka])
                        for j in range(8):
                            P.op('pe', lambda e, j=j, cc=cc, pb_=pb_, tsl=tsl: e.matmul(pb_[:], lhsT=wg[:, j, 512 + cc * 128:512 + (cc + 1) * 128], rhs=hT[:, j, tsl],
                                                                               start=(j == 0), stop=(j == 7)),
                                 reads=['wg'] + hkeys, writes=[kb])
                        sg = sig[cc % 2]
                        P.op('act', lambda e, pb_=pb_, sg=sg: e.activation(out=sg[:], in_=pb_[:], func=AF.Sigmoid),
                             reads=[kb], writes=[('sig', cc % 2)])
                        P.op('dve', lambda e, cc=cc, pa_=pa_, sg=sg, tc=tc: e.tensor_tensor(out=uT[:, cc, 30 + tc * 512:30 + (tc + 1) * 512], in0=pa_[:], in1=sg[:], op=ALU.mult),
                             reads=[ka, ('sig', cc % 2)], writes=[('uT', cc, tc)])
                    for cc in range(4):
                        py = psb[4 + cc % 2]
                        ky = ('ps', 4 + cc % 2)
                        rd = [('uT', cc, tc), ('diag', cc), 'uTpad'] + ([('uT', cc, tc - 1)] if tc > 0 else [])
                        for k in range(31):
                            P.op('pe', lambda e, cc=cc, k=k, py=py, tc=tc: e.matmul(py[:], lhsT=diag[:, cc * 31 + k, :],
                                                                                  rhs=uT[:, cc, tc * 512 + k:tc * 512 + k + 512],
                                                                                  start=(k == 0), stop=(k == 30)),
                                 reads=rd, writes=[ky])
                        P.op('dve', lambda e, cc=cc, py=py: e.tensor_scalar(out=ysb[:, cc, :], in0=py[:], scalar1=cvec[:, 0, cc:cc + 1], scalar2=None, op0=ALU.add),
                             reads=[ky, 'cvec'], writes=[('ysb', cc)])
                        P.op('act', lambda e, cc=cc: e.activation(out=ysq[:, cc, :], in_=ysb[:, cc, :], func=AF.Square),
                             reads=[('ysb', cc)], writes=[('ysq', cc)])
                    pm, pe2 = psb[6], psb[7]
                    for cc in range(4):
                        P.op('pe', lambda e, cc=cc: e.matmul(pm[:], lhsT=onesk[:], rhs=ysb[:, cc, :], start=(cc == 0), stop=(cc == 3)),
                             reads=['onesk', ('ysb', cc)], writes=[('ps', 6)])
                    for cc in range(4):
                        P.op('pe', lambda e, cc=cc: e.matmul(pe2[:], lhsT=onesk[:], rhs=ysq[:, cc, :], start=(cc == 0), stop=(cc == 3)),
                             reads=['onesk', ('ysq', cc)], writes=[('ps', 7)])
                    P.op('act', lambda e: e.activation(out=msq[:], in_=pm[:], func=AF.Square), reads=[('ps', 6)], writes=['msq'])
                    P.op('dve', lambda e: e.tensor_tensor(out=var[:], in0=pe2[:], in1=msq[:], op=ALU.subtract), reads=[('ps', 7), 'msq'], writes=['var'])
                    P.op('dve', lambda e: e.tensor_scalar(out=var[:], in0=var[:], scalar1=0.0, scalar2=EPS, op0=ALU.max, op1=ALU.add), reads=['var'], writes=['var'])
                    P.op('act', lambda e: e.activation(out=var[:], in_=var[:], func=AF.Sqrt), reads=['var'], writes=['var'])
                    P.op('dve', lambda e: e.reciprocal(out=rstdc[:], in_=var[:]), reads=['var'], writes=['rstdc'])
                    for cc in range(4):
                        tb = t1[cc % 2]
                        P.op('dve', lambda e, cc=cc, tb=tb: e.tensor_tensor(out=tb[:], in0=ysb[:, cc, :], in1=pm[:], op=ALU.subtract),
                             reads=[('ysb', cc), ('ps', 6)], writes=[('t1', cc % 2)])
                        P.op('pool', lambda e, tb=tb: e.tensor_tensor(out=tb[:], in0=tb[:], in1=rstdc[:], op=ALU.mult),
                             reads=[('t1', cc % 2), 'rstdc'], writes=[('t1', cc % 2)])
                        P.op('act', lambda e, cc=cc, tb=tb, tsl=tsl: e.activation(out=c_outT[:, cc, tsl], in_=tb[:], func=AF.Silu,
                                                                         scale=cvec[:, 1, cc:cc + 1], bias=cvec[:, 2, cc:cc + 1]),
                             reads=[('t1', cc % 2), 'cvec'], writes=[('cout', cc, tc)])
                cout_all = [('cout', cc, tc) for cc in range(4) for tc in range(4)]
                if debug:
                    cof = sbt(pb, "cof", [128, 4, 512], F32)
                    for tc in range(4):
                        if os.environ.get('DBG_U'):
                            P.op('dve', lambda e, tc=tc: e.tensor_copy(out=cof[:], in_=uT[:, :, 30 + tc * 512:30 + (tc + 1) * 512]), reads=cout_all, writes=['cof'])
                        else:
                            P.op('dve', lambda e, tc=tc: e.tensor_copy(out=cof[:], in_=c_outT[:, :, tc * 512:(tc + 1) * 512]), reads=cout_all, writes=['cof'])
                        P.dma('sp', lambda e, tc=tc: e.dma_start(out=dbg['cout'][:, :, tc * 512:(tc + 1) * 512], in_=cof[:]), 'dbg', reads=['cof'])
                P.barrier()
                P.flush()
            if stage <= 2:
                return nc
            with contextlib.ExitStack() as pc:
                wkv = sbt(pc, "wkv", [128, 8, 768], BF16)
                wqg = sbt(pc, "wqg", [128, 8, 536], BF16)
                w_o = sbt(pc, "w_o", [128, 8, D], BF16)
                kT = sbt(pc, "kT", [64, 4, S], BF16)
                vaug = sbt(pc, "vaug", [128, 16, 4, 65], BF16)
                kAB = sbt(pc, "kAB", [64, 2, S], BF16)
                petab = sbt(pc, "petab", [64, 4, 512], F32)
                wc = sbt(pc, "wc", [64, 2, 32, 64], BF16)
                qkg = sbt(pc, "qkg", [128, 4, 64], F32)
                sqk = sbt(pc, "sqk", [128, 512], F32)
                ssk = sbt(pc, "ssk", [128, 8], F32)
                rstdk = sbt(pc, "rstdk", [128, 8], F32)
                tmpk = sbt(pc, "tmpk", [128, 512], F32)
                kn = sbt(pc, "kn", [128, 512], BF16)
                kcnT = sbt(pc, "kcnT", [64, 2, 128], BF16)
                vcaug = sbt(pc, "vcaug", [128, 2, 97], BF16)
                kcn = sbt(pc, "kcn", [128, 64], BF16)
                cmaskT = sbt(pc, "cmaskT", [128, S], BF16)
                tri = sbt(pc, "tri", [128, 2, 128], BF16)
                esel = sbt(pc, "esel", [32, 16, 128], BF16)
                biasw = sbt(pc, "biasw", [128, 8, 16], F32)
                biasc = sbt(pc, "biasc", [128, 8, 16], F32)
                validm = sbt(pc, "validm", [128, 8, 32], F32)
                selb = sbt(pc, "selb", [128, 8, 32], F32)
                small = sbt(pc, "small", [128, 64], F32)

                for j in range(8):
                    P.dma('pool', lambda e, j=j: e.dma_start(out=wkv[:, j, :], in_=win_v[:, j, 512:1280]), 'wkv', writes=['wkv'])
                for j in range(8):
                    P.dma('pool', lambda e, j=j: e.dma_start(out=wqg[:, j, 0:512], in_=win_v[:, j, 0:512]), 'wqg', writes=['wqg'])
                    P.dma('pool', lambda e, j=j: e.dma_start(out=wqg[:, j, 512:536], in_=win_v[:, j, 1280:1304]), 'wqg', writes=['wqg'])
                wout_v = dr['w_out'].rearrange("(j p) c -> p j c", p=128)
                for j in range(8):
                    P.dma('pool', lambda e, j=j: e.dma_start(out=w_o[:, j, :], in_=wout_v[:, j, :]), 'wo', writes=['w_o'])
                P.dma('pool', lambda e: e.dma_start(out=wc[:, 0, :, :], in_=dr['wck']), 'wc', writes=['wc'])
                P.dma('pool', lambda e: e.dma_start(out=wc[:, 1, :, :], in_=dr['wcv']), 'wc', writes=['wc'])
                P.dma('pool', lambda e: e.dma_start(out=cmaskT[:], in_=dr['cmaskT']), 'cst', writes=['cst'])
                P.dma('pool', lambda e: e.dma_start(out=tri[:], in_=dr['tri']), 'cst', writes=['cst'])
                P.dma('pool', lambda e: e.dma_start(out=esel[:], in_=dr['esel']), 'cst', writes=['cst'])
                P.op('pool', lambda e: e.memset(vcaug[:], 0.0), writes=['vcaug0'])
                P.op('pool', lambda e: e.memset(vcaug[:, :, 64:65], 1.0), reads=['vcaug0'], writes=['vcaug1'])
                for g in range(2):
                    P.dma('pool', lambda e, g=g: e.dma_start(out=vcaug[:, g, 65:97], in_=dr['ovl']), 'cst', reads=['vcaug0'], writes=['cst'])
                P.op('dve', lambda e: e.memset(vaug[:, :, :, 64:65], 1.0), writes=['vaug1'])
                P.dma('sp', lambda e: e.dma_start(out=petab[:], in_=dr['pe_tab']), 'c6', writes=['petab'])
                P.dma('sp', lambda e: e.dma_start(out=qkg[:], in_=dr['qkg_bc']), 'c6', writes=['qkg'])
                P.dma('sp', lambda e: e.dma_start(out=biasw[:], in_=dr['biasw']), 'c6', writes=['cst2'])
                P.dma('sp', lambda e: e.dma_start(out=biasc[:], in_=dr['biasc']), 'c6', writes=['cst2'])
                P.dma('sp', lambda e: e.dma_start(out=validm[:], in_=dr['validm']), 'c6', writes=['cst2'])
                P.dma('sp', lambda e: e.dma_start(out=selb[:], in_=dr['selb']), 'c6', writes=['cst2'])

                def rms_heads(ps_ap, nblk, key_in, wkey):
                    P.op('act', lambda e: e.activation(out=sqk[:, 0:nblk * 64], in_=ps_ap, func=AF.Square), reads=[key_in], writes=['sqk'])
                    P.op('dve', lambda e: e.tensor_reduce(out=ssk[:, 0:nblk], in_=sqk[:, 0:nblk * 64].rearrange("p (a b) -> p a b", b=64),
                                                          axis=AX.X, op=ALU.add), reads=['sqk'], writes=['ssk'])
                    P.op('dve', lambda e: e.tensor_scalar(out=ssk[:, 0:nblk], in0=ssk[:, 0:nblk], scalar1=1.0 / 64, scalar2=EPS,
                                                          op0=ALU.mult, op1=ALU.add), reads=['ssk'], writes=['ssk'])
                    rsqrt_pool(rstdk[:, 0:nblk], ssk[:, 0:nblk], nblk, ['ssk'], [wkey])

                ssk2 = sbt(pc, "ssk2", [128, 2, 8], F32)
                rstdk2 = sbt(pc, "rstdk2", [128, 2, 8], F32)

                def kv_stage_a(tt):
                    p = tt % 2
                    pk = psb[0] if p == 0 else psb[6]
                    pkk = ('ps', 0 if p == 0 else 6)
                    for j in range(8):
                        P.op('pe', lambda e, j=j: e.matmul(pk[:], lhsT=hT[:, j, tt * 128:(tt + 1) * 128], rhs=wkv[:, j, 256:768],
                                                           start=(j == 0), stop=(j == 7)),
                             reads=[('hT', tt), 'wkv'], writes=[pkk])
                    P.op('act', lambda e: e.activation(out=sqk[:], in_=pk[:], func=AF.Square), reads=[pkk], writes=['sqk'])
                    P.op('dve', lambda e: e.tensor_reduce(out=ssk2[:, p, :], in_=sqk[:].rearrange("p (a b) -> p a b", b=64), axis=AX.X, op=ALU.add),
                         reads=['sqk'], writes=[('ssk2', p)])
                    P.op('dve', lambda e: e.tensor_scalar(out=ssk2[:, p, :], in0=ssk2[:, p, :], scalar1=1.0 / 64, scalar2=EPS, op0=ALU.mult, op1=ALU.add),
                         reads=[('ssk2', p)], writes=[('ssk2', p)])
                    P.op('act', lambda e: e.activation(out=ssk2[:, p, :], in_=ssk2[:, p, :], func=AF.Sqrt), reads=[('ssk2', p)], writes=[('ssk2', p)])
                    P.op('dve', lambda e: e.reciprocal(out=rstdk2[:, p, :], in_=ssk2[:, p, :]), reads=[('ssk2', p)], writes=[('rstdk2', p)])
                    P.op('act', lambda e: e.activation(out=vaug[:, tt, 0:2, 0:64], in_=pk[:, 128:256].rearrange("p (a b) -> p a b", b=64), func=AF.Copy),
                         reads=[pkk, 'vaug1'], writes=[('vaug', tt)])
                    P.op('act', lambda e: e.activation(out=vaug[:, tt, 2:4, 0:64], in_=pk[:, 384:512].rearrange("p (a b) -> p a b", b=64), func=AF.Copy),
                         reads=[pkk, 'vaug1'], writes=[('vaug', tt)])

                def kv_stage_b(tt):
                    p = tt % 2
                    pk = psb[0] if p == 0 else psb[6]
                    pkk = ('ps', 0 if p == 0 else 6)
                    for bi, (blk, gi) in enumerate([(0, 2), (4, 3)]):
                        P.op('dve', lambda e, blk=blk, bi=bi: e.tensor_tensor(
                            out=tmpk[:, bi * 128:(bi + 1) * 128].rearrange("p (a b) -> p a b", b=64),
                            in0=pk[:, blk * 64:(blk + 2) * 64].rearrange("p (a b) -> p a b", b=64),
                            in1=rstdk2[:, p, blk:blk + 2].unsqueeze(2).broadcast_to([128, 2, 64]), op=ALU.mult),
                            reads=[pkk, ('rstdk2', p)], writes=[('tmpk', bi)])
                        P.op('dve', lambda e, bi=bi, gi=gi: e.tensor_tensor(
                            out=kn[:, bi * 128:(bi + 1) * 128].rearrange("p (a b) -> p a b", b=64),
                            in0=tmpk[:, bi * 128:(bi + 1) * 128].rearrange("p (a b) -> p a b", b=64),
                            in1=qkg[:, gi:gi + 1, :].broadcast_to([128, 2, 64]), op=ALU.mult),
                            reads=[('tmpk', bi), 'qkg'], writes=[('kn', bi)])
                    pT = psb[1][:].bitcast(BF16)
                    for i4 in range(4):
                        P.op('pe', lambda e, i4=i4: e.transpose(out=pT[0:64, i4 * 128:(i4 + 1) * 128], in_=kn[:, i4 * 64:(i4 + 1) * 64], identity=ident[:]),
                             reads=[('kn', i4 // 2), 'ident'], writes=[('ps', 1)])
                    P.op('act', lambda e: e.activation(out=kT[:, :, tt * 128:(tt + 1) * 128],
                                                       in_=pT[0:64, 0:512].rearrange("p (a b) -> p a b", b=128), func=AF.Copy),
                         reads=[('ps', 1)], writes=[('kT', tt)])

                kv_stage_a(0)
                for tt in range(NT):
                    if tt + 1 < NT:
                        kv_stage_a(tt + 1)
                    kv_stage_b(tt)
                for which in range(2):
                    for g in range(2):
                        for tc in range(4):
                            pf_ = psb[2 + tc % 2]
                            for j in range(8):
                                P.op('pe', lambda e, j=j, tc=tc, pf_=pf_, which=which, g=g: e.matmul(
                                    pf_[0:64, :], lhsT=wkv[:, j, which * 128 + g * 64:which * 128 + (g + 1) * 64], rhs=hT[:, j, tc * 512:(tc + 1) * 512],
                                    start=(j == 0), stop=(j == 7)), reads=hT_all + ['wkv'], writes=[('ps', 2 + tc % 2)])
                            for ab in range(2):
                                P.op('dve', lambda e, tc=tc, pf_=pf_, ab=ab, which=which: e.tensor_tensor(
                                    out=kAB[:, ab, tc * 512:(tc + 1) * 512], in0=pf_[0:64, :], in1=petab[:, which * 2 + ab, :], op=ALU.add),
                                    reads=[('ps', 2 + tc % 2), 'petab'], writes=[('kAB', tc)])
                        pcmp = psb[4]
                        for l in range(32):
                            ab = 0 if l < 16 else 1
                            P.op('pe', lambda e, l=l, ab=ab, which=which: e.matmul(
                                pcmp[0:127, 0:64], lhsT=kAB[:, ab, l:l + 16 * 126 + 1:16], rhs=wc[:, which, l, :], start=(l == 0), stop=(l == 31)),
                                reads=[('kAB', tc) for tc in range(4)] + ['wc'], writes=[('ps', 4)])
                        if which == 0:
                            P.op('act', lambda e: e.activation(out=sqk[0:127, 0:64], in_=pcmp[0:127, 0:64], func=AF.Square, accum_out=small[0:127, 0:1]),
                                 reads=[('ps', 4)], writes=['sqk', 'small'])
                            P.op('dve', lambda e: e.tensor_scalar(out=small[0:127, 1:2], in0=small[0:127, 0:1], scalar1=1.0 / 64, scalar2=EPS,
                                                                  op0=ALU.mult, op1=ALU.add), reads=['small'], writes=['small'])
                            rsqrt_pool(small[0:127, 2:3], small[0:127, 1:2], 1, ['small'], ['small'])
                            P.op('dve', lambda e: e.scalar_tensor_tensor(out=kcn[0:127, :], in0=pcmp[0:127, 0:64], scalar=small[0:127, 2:3],
                                                                         in1=qkg[0:127, 1, :], op0=ALU.mult, op1=ALU.mult),
                                 reads=[('ps', 4), 'small', 'qkg'], writes=['kcn'])
                            pT5 = psb[5][:].bitcast(BF16)
                            P.op('pe', lambda e: e.transpose(out=pT5[0:64, 0:127], in_=kcn[0:127, :], identity=ident[0:127, 0:127]),
                                 reads=['kcn', 'ident'], writes=[('ps', 5)])
                            P.op('act', lambda e, g=g: e.activation(out=kcnT[:, g, 0:127], in_=pT5[0:64, 0:127], func=AF.Copy),
                                 reads=[('ps', 5)], writes=[('kcnT', g)])
                        else:
                            P.op('act', lambda e, g=g: e.activation(out=vcaug[0:127, g, 0:64], in_=pcmp[0:127, 0:64], func=AF.Copy),
                                 reads=[('ps', 4), 'vcaug0'], writes=[('vcaug', g)])
                if debug and stage == 3:
                    kTf = sbt(pc, "kTf", [64, 4, 512], F32)
                    for q4 in range(4):
                        P.op('dve', lambda e, q4=q4: e.tensor_copy(out=kTf[:], in_=kT[:, :, q4 * 512:(q4 + 1) * 512]), reads=[('kT', t) for t in range(NT)], writes=['kTf'])
                        P.dma('sp', lambda e, q4=q4: e.dma_start(out=dbg['kT'][:, :, q4 * 512:(q4 + 1) * 512], in_=kTf[:]), 'dbg', reads=['kTf'])
                    vaf = sbt(pc, "vaf", [128, 16 * 4 * 65], F32)
                    P.op('dve', lambda e: e.tensor_copy(out=vaf[:], in_=vaug[:].rearrange("p a b c -> p (a b c)")), reads=[('vaug', t) for t in range(NT)] + ['vaug1'], writes=['vaf'])
                    P.dma('sp', lambda e: e.dma_start(out=dbg['vaug'], in_=vaf[:]), 'dbg', reads=['vaf'])
                    kcf = sbt(pc, "kcf", [64, 2 * 128], F32)
                    P.op('dve', lambda e: e.tensor_copy(out=kcf[:, 0:127], in_=kcnT[:, 0, 0:127]), reads=[('kcnT', 0)], writes=['kcf'])
                    P.op('dve', lambda e: e.tensor_copy(out=kcf[:, 128:255], in_=kcnT[:, 1, 0:127]), reads=[('kcnT', 1)], writes=['kcf'])
                    P.dma('sp', lambda e: e.dma_start(out=dbg['kcnT'], in_=kcf[:]), 'dbg', reads=['kcf'])
                    vcf = sbt(pc, "vcf", [128, 2 * 97], F32)
                    P.op('dve', lambda e: e.tensor_copy(out=vcf[:], in_=vcaug[:].rearrange("p a b -> p (a b)")), reads=[('vcaug', 0), ('vcaug', 1), 'vcaug1', 'cst'], writes=['vcf'])
                    P.dma('sp', lambda e: e.dma_start(out=dbg['vcaug'], in_=vcf[:]), 'dbg', reads=['vcf'])
                P.barrier()
                P.flush()
                if stage <= 3:
                    return nc

                qTt = sbt(pc, "qTt", [64, 8, 128], BF16)
                gates = sbt(pc, "gates", [128, 24], F32)
                qn = sbt(pc, "qn", [128, 512], BF16)
                pTs = [sbt(pc, "pT%d" % i, [128, 128], BF16) for i in range(8)]
                acc = sbt(pc, "acc", [128, 8, 64], F32)
                aout = sbt(pc, "aout", [128, 512], BF16)
                catT = sbt(pc, "catT", [128, 4, 128], BF16)
                impa = sbt(pc, "impa", [128, 32], F32)
                adj = sbt(pc, "adj", [128, 32], F32)
                adj2 = sbt(pc, "adj2", [128, 32], F32)
                top8 = sbt(pc, "top8", [128, 16], F32)
                negsel = sbt(pc, "negsel", [128, 32], BF16)
                negselT = sbt(pc, "negselT", [32, 128], BF16)
                dens = sbt(pc, "dens", [128, 8, 8], F32)
                xr = [sbt(pc, "xr%d" % i, [128, D], F32) for i in range(2)]
                x1t = [sbt(pc, "x1t%d" % i, [128, D], F32) for i in range(2)]
                if debug:
                    aof = sbt(pc, "aof", [128, 512], F32)
                kT_all = [('kT', t) for t in range(NT)]
                va_all = [('vaug', t) for t in range(NT)]

                qTt2 = [qTt, sbt(pc, "qTtB", [64, 8, 128], BF16)]
                gates2 = [gates, sbt(pc, "gatesB", [128, 24], F32)]

                def qprep(qt):
                    qsl = slice(qt * 128, (qt + 1) * 128)
                    qT_ = qTt2[qt % 2]
                    gt_ = gates2[qt % 2]
                    kq = ('qTt', qt % 2)
                    kg = ('gates', qt % 2)
                    pq = psb[0]
                    for j in range(8):
                        P.op('pe', lambda e, j=j: e.matmul(pq[:], lhsT=hT[:, j, qsl], rhs=wqg[:, j, 0:512], start=(j == 0), stop=(j == 7)),
                             reads=[('hT', qt), 'wqg'], writes=[('ps', 0)])
                    pg = psb[1]
                    for j in range(8):
                        P.op('pe', lambda e, j=j: e.matmul(pg[:, 0:24], lhsT=hT[:, j, qsl], rhs=wqg[:, j, 512:536], start=(j == 0), stop=(j == 7)),
                             reads=[('hT', qt), 'wqg'], writes=[('ps', 1)])
                    P.op('act', lambda e: e.activation(out=gt_[:], in_=pg[:, 0:24], func=AF.Sigmoid), reads=[('ps', 1)], writes=[kg])
                    rms_heads(pq[:], 8, ('ps', 0), 'rstdq')
                    P.op('dve', lambda e: e.tensor_tensor(out=tmpk[:].rearrange("p (a b) -> p a b", b=64), in0=pq[:].rearrange("p (a b) -> p a b", b=64),
                                                          in1=rstdk[:, 0:8].unsqueeze(2).broadcast_to([128, 8, 64]), op=ALU.mult),
                         reads=[('ps', 0), 'rstdq'], writes=['tmpq'])
                    P.op('dve', lambda e: e.tensor_tensor(out=qn[:].rearrange("p (a b) -> p a b", b=64), in0=tmpk[:].rearrange("p (a b) -> p a b", b=64),
                                                          in1=qkg[:, 0:1, :].broadcast_to([128, 8, 64]), op=ALU.mult),
                         reads=['tmpq', 'qkg'], writes=['qn'])

                def qprep_b(qt):
                    qT_ = qTt2[qt % 2]
                    kq = ('qTt', qt % 2)
                    pT1 = psb[1][:].bitcast(BF16)
                    for h in range(8):
                        P.op('pe', lambda e, h=h: e.transpose(out=pT1[0:64, h * 128:(h + 1) * 128], in_=qn[:, h * 64:(h + 1) * 64], identity=ident[:]),
                             reads=['qn', 'ident'], writes=[('ps', 1)])
                    P.op('act', lambda e: e.activation(out=qT_[:], in_=pT1[0:64, :].rearrange("p (a b) -> p a b", b=128), func=AF.Copy),
                         reads=[('ps', 1)], writes=[kq])
                    P.dma('pool', lambda e, tt=qt: e.dma_start(out=uvt[tt * 1024:(tt + 1) * 1024, 0:D], in_=dr['peer_u'][tt * 1024:(tt + 1) * 1024, :]), 'uvt', writes=['uvt'])
                    P.dma('pool', lambda e, tt=qt: e.dma_start(out=uvt[tt * 1024:(tt + 1) * 1024, D:2 * D], in_=dr['peer_v'][tt * 1024:(tt + 1) * 1024, :]), 'uvt', writes=['uvt'])

                def tile_tail(qt):
                    qsl = slice(qt * 128, (qt + 1) * 128)
                    aokeys = [('aout', h) for h in range(8)]
                    if debug:
                        P.op('dve', lambda e: e.tensor_copy(out=aof[:], in_=aout[:]), reads=aokeys, writes=['aof'])
                        P.dma('sp', lambda e: e.dma_start(out=dbg['aout'][qsl, :], in_=aof[:]), 'dbg2', reads=['aof'])
                    pT1 = psb[1][:].bitcast(BF16)
                    for j in range(4):
                        P.op('pe', lambda e, j=j: e.transpose(out=pT1[:, j * 128:(j + 1) * 128], in_=aout[:, j * 128:(j + 1) * 128], identity=ident[:]),
                             reads=aokeys + ['ident'], writes=[('ps', 1)])
                    P.op('act', lambda e: e.activation(out=catT[:], in_=pT1[:, 0:512].rearrange("p (a b) -> p a b", b=128), func=AF.Copy),
                         reads=[('ps', 1)], writes=['catT'])
                    b = qt % 2
                    P.dma('sp', lambda e: e.dma_start(out=xr[b][:], in_=dr['x'][qsl, :]), 'xr%d' % b, writes=[('xr', b)])
                    for half in range(2):
                        pm_ = psb[half]
                        hs = slice(half * 512, (half + 1) * 512)
                        for j in range(4):
                            P.op('pe', lambda e, j=j, pm_=pm_, hs=hs: e.matmul(pm_[:], lhsT=catT[:, j, :], rhs=w_o[:, j, hs], start=(j == 0), stop=False),
                                 reads=['catT', 'w_o'], writes=[('ps', half)])
                        for j in range(4):
                            P.op('pe', lambda e, j=j, pm_=pm_, hs=hs: e.matmul(pm_[:], lhsT=c_outT[:, j, qsl], rhs=w_o[:, 4 + j, hs], start=False, stop=(j == 3)),
                                 reads=['w_o'], writes=[('ps', half)])
                        P.op('dve', lambda e, pm_=pm_, hs=hs: e.tensor_tensor(out=x1t[b][:, hs], in0=pm_[:], in1=g_m[:, hs], op=ALU.mult),
                             reads=[('ps', half)], writes=[('x1t', b, half)])
                        P.op('dve', lambda e, hs=hs: e.tensor_tensor(out=x1t[b][:, hs], in0=x1t[b][:, hs], in1=xr[b][:, hs], op=ALU.add),
                             reads=[('x1t', b, half), ('xr', b)], writes=[('x1t', b, half)])
                    P.dma('sp', lambda e: e.dma_start(out=x1s[qsl, :], in_=x1t[b][:]), 'x1o%d' % b,
                          reads=[('x1t', b, 0), ('x1t', b, 1)], writes=[('x1s', qt)])

                for qt in range(int(os.environ.get("NQT", NT))):
                    qsl = slice(qt * 128, (qt + 1) * 128)
                    if qt == 0:
                        qprep(0)
                        qprep_b(0)
                    qTt = qTt2[qt % 2]
                    gates = gates2[qt % 2]

                    for g in range(int(os.environ.get('NG', 2))):
                        jobs = []

                        def cmp_post(h, r, g=g, qt=qt):
                            gates = gates2[qt % 2]
                            kgt = ('gates', qt % 2)
                            ob = psb[2 + h % 2]
                            ok = ('ps', 2 + h % 2)
                            P.op('dve', lambda e: e.tensor_scalar(out=dens[:, h, 0:1], in0=ob[:, 64:65], scalar1=1e-30, scalar2=None, op0=ALU.max),
                                 reads=[ok], writes=[('dens', h)])
                            P.op('dve', lambda e: e.reciprocal(out=dens[:, h, 1:2], in_=dens[:, h, 0:1]), reads=[('dens', h)], writes=[('dens', h)])
                            P.op('dve', lambda e: e.tensor_tensor(out=dens[:, h, 2:3], in0=dens[:, h, 1:2], in1=gates[:, h * 3:h * 3 + 1], op=ALU.mult),
                                 reads=[('dens', h), kgt], writes=[('dens', h)])
                            P.op('dve', lambda e: e.tensor_scalar(out=acc[:, h, :], in0=ob[:, 0:64], scalar1=dens[:, h, 2:3], scalar2=None, op0=ALU.mult),
                                 reads=[ok, ('dens', h)], writes=[('acc', h)])
                            if qt >= 8:
                                if r == 0:
                                    P.op('dve', lambda e: e.tensor_scalar(out=impa[:], in0=ob[:, 65:97], scalar1=dens[:, h, 1:2], scalar2=None, op0=ALU.mult),
                                         reads=[ok, ('dens', h)], writes=['impa'])
                                else:
                                    P.op('dve', lambda e: e.scalar_tensor_tensor(out=impa[:], in0=ob[:, 65:97], scalar=dens[:, h, 1:2], in1=impa[:],
                                                                                 op0=ALU.mult, op1=ALU.add),
                                         reads=[ok, ('dens', h), 'impa'], writes=['impa'])
                                if r == 3:
                                    P.op('dve', lambda e: e.tensor_tensor(out=adj[:], in0=impa[:], in1=validm[:, qt - 8, :], op=ALU.mult),
                                         reads=['impa', 'cst2'], writes=['adj'])
                                    P.op('dve', lambda e: e.tensor_tensor(out=adj[:], in0=adj[:], in1=selb[:, qt - 8, :], op=ALU.add),
                                         reads=['adj', 'cst2'], writes=['adj'])
                                    P.op('dve', lambda e: e.max(out=top8[:, 0:8], in_=adj[:]), reads=['adj'], writes=['top8'])
                                    P.op('dve', lambda e: e.match_replace(out=adj2[:], in_to_replace=top8[:, 0:8], in_values=adj[:], imm_value=-1e30),
                                         reads=['adj', 'top8'], writes=['adj2'])
                                    P.op('dve', lambda e: e.max(out=top8[:, 8:16], in_=adj2[:]), reads=['adj2'], writes=['top8b'])
                                    P.op('dve', lambda e: e.tensor_scalar(out=negsel[:], in0=adj[:], scalar1=top8[:, 15:16], scalar2=NEG,
                                                                          op0=ALU.is_lt, op1=ALU.mult),
                                         reads=['adj', 'top8b'], writes=['negsel'])
                                    pT7 = psb[1][:].bitcast(BF16)
                                    P.op('pe', lambda e: e.transpose(out=pT7[0:32, 0:128], in_=negsel[:], identity=ident[:]),
                                         reads=['negsel', 'ident'], writes=[('ps', 1)])
                                    P.op('act', lambda e: e.activation(out=negselT[:], in_=pT7[0:32, 0:128], func=AF.Copy),
                                         reads=[('ps', 1)], writes=['negselT'])

                        def win_post(h, g=g, qt=qt):
                            gates = gates2[qt % 2]
                            kgt = ('gates', qt % 2)
                            ob = psb[2 + h % 2]
                            ok = ('ps', 2 + h % 2)
                            P.op('dve', lambda e: e.reciprocal(out=dens[:, h, 5:6], in_=ob[:, 320:321]), reads=[ok], writes=[('dens', h)])
                            P.op('dve', lambda e: e.tensor_tensor(out=dens[:, h, 7:8], in0=dens[:, h, 5:6], in1=gates[:, h * 3 + 2:h * 3 + 3], op=ALU.mult),
                                 reads=[('dens', h), kgt], writes=[('dens', h)])
                            P.op('dve', lambda e: e.scalar_tensor_tensor(out=acc[:, h, :], in0=ob[:, 256:320], scalar=dens[:, h, 7:8], in1=acc[:, h, :],
                                                                         op0=ALU.mult, op1=ALU.add),
                                 reads=[ok, ('dens', h), ('acc', h)], writes=[('acc', h)])

                        def fin_post(h, g=g, qt=qt):
                            gates = gates2[qt % 2]
                            kgt = ('gates', qt % 2)
                            ob = psb[2 + h % 2]
                            ok = ('ps', 2 + h % 2)
                            P.op('dve', lambda e: e.reciprocal(out=dens[:, h, 4:5], in_=ob[:, 192:193]), reads=[ok], writes=[('dens', h)])
                            P.op('dve', lambda e: e.tensor_tensor(out=dens[:, h, 6:7], in0=dens[:, h, 4:5], in1=gates[:, h * 3 + 1:h * 3 + 2], op=ALU.mult),
                                 reads=[('dens', h), kgt], writes=[('dens', h)])
                            P.op('dve', lambda e: e.scalar_tensor_tensor(out=aout[:, h * 64:(h + 1) * 64], in0=ob[:, 128:192], scalar=dens[:, h, 6:7],
                                                                         in1=acc[:, h, :], op0=ALU.mult, op1=ALU.add),
                                 reads=[ok, ('dens', h), ('acc', h)], writes=[('aout', h)])

                        for r in range(4):
                            h = g * 4 + r
                            jobs.append(dict(kind='cmp', h=h, r=r, nk=127, lhsT=kcnT[:, g, 0:127], lk=[('kcnT', g)],
                                             mask=('id', cmaskT[:, qsl]), bias=biasc[0:127, h, qt:qt + 1],
                                             v=vcaug[0:127, g, :], vk=[('vcaug', g), 'vcaug1', 'cst'], oc=(0, 97), obk=2 + h % 2, start=True, stop=True,
                                             post=(lambda h=h, r=r: cmp_post(h, r))))
                        win_jobs, slc_jobs = [], []
                        for r in range(4):
                            h = g * 4 + r
                            sl_h = float(alibi_slopes()[h])
                            skip_from = 99
                            for dl_ in range(1, 17):
                                if sl_h * (128.0 * (dl_ - 1) + 1.0) >= 60.0:
                                    skip_from = dl_
                                    break
                            wl = [kc for kc in range(max(0, qt - 4), qt + 1) if qt - kc < skip_from]
                            if os.environ.get('SKIPWIN'):
                                wl = []
                            for i, kc in enumerate(wl):
                                mask = None
                                if kc == qt:
                                    mask = ('id', tri[:, 0, :])
                                elif kc == qt - 4:
                                    mask = ('id', tri[:, 1, :])
                                win_jobs.append(dict(kind='win', h=h, r=r, nk=128, lhsT=kT[:, 2 + g, kc * 128:(kc + 1) * 128], lk=[('kT', kc)],
                                                 mask=mask, bias=biasw[:, h, qt - kc:qt - kc + 1], v=vaug[:, kc, 2 + g, :], vk=[('vaug', kc), 'vaug1'],
                                                 oc=(256, 321), obk=2 + h % 2, start=(i == 0), stop=(i == len(wl) - 1),
                                                 post=((lambda h=h: win_post(h)) if i == len(wl) - 1 else None)))
                            klist = [kc for kc in range(qt + 1) if qt - kc < skip_from]
                            for kc in klist:
                                mask = None
                                if kc == qt:
                                    mask = ('id', tri[:, 0, :])
                                elif qt >= 8:
                                    mask = ('sel', esel[:, kc, :])
                                gx = g + (2 if os.environ.get('E1') else 0)
                                slc_jobs.append(dict(kind='slc', h=h, r=r, nk=128, lhsT=kT[:, gx, kc * 128:(kc + 1) * 128], lk=[('kT', kc)],
                                                 mask=mask, bias=biasw[:, h, qt - kc:qt - kc + 1], v=vaug[:, kc, gx, :], vk=[('vaug', kc), 'vaug1'],
                                                 oc=(128, 193), obk=2 + h % 2, start=(kc == klist[0]), stop=(kc == qt),
                                                 post=((lambda h=h: fin_post(h)) if kc == qt else None)))
                        jobs = jobs + win_jobs + slc_jobs
                        LA = int(os.environ.get('LA', 3))
                        jobs = jobs[:int(os.environ.get('NJOBS', 100000))]
                        for i in range(len(jobs) + LA):
                            k2 = i - LA
                            if k2 >= 0 and not os.environ.get('NOPV'):
                                jb = jobs[k2]
                                sl_ = k2 % 8
                                sp_ = psb[4 + sl_ % 4][0:jb['nk'], 0:128]
                                skey = ('S', sl_ % 4)
                                pt_ = pTs[sl_]
                                P.op('act', lambda e, jb=jb, sp_=sp_, pt_=pt_: e.activation(out=pt_[0:jb['nk'], :], in_=sp_, func=AF.Exp,
                                                                                          bias=jb['bias'], scale=0.125),
                                     reads=[skey, 'cst2'], writes=[('pT', sl_)])
                                ob = psb[jb['obk']]
                                c0, c1 = jb['oc']
                                P.op('pe', lambda e, jb=jb, pt_=pt_, ob=ob, c0=c0, c1=c1: e.matmul(ob[:, c0:c1], lhsT=pt_[0:jb['nk'], :], rhs=jb['v'],
                                                                                                  start=jb['start'], stop=jb['stop']),
                                     reads=[('pT', sl_)] + jb['vk'], writes=[('ps', jb['obk'])])
                                if jb['post'] is not None and not os.environ.get('NOPOST'):
                                    jb['post']()
                            if i < len(jobs):
                                jb = jobs[i]
                                sl_ = i % 8
                                sp_ = psb[4 + sl_ % 4][0:jb['nk'], 0:128]
                                skey = ('S', sl_ % 4)
                                bkey = ('ps', 4 + sl_ // 4)
                                hm = jb['mask'] is not None and not os.environ.get('NOMASK')
                                P.op('pe', lambda e, jb=jb, sp_=sp_, hm=hm, qq=qTt2[qt % 2]: e.matmul(sp_, lhsT=jb['lhsT'], rhs=qq[:, jb['h'], :], start=True, stop=not hm),
                                     reads=jb['lk'] + [('qTt', qt % 2)], writes=[skey])
                                if g == 0 and i == 4 and qt >= 1:
                                    tile_tail(qt - 1)
                                if g == 0 and i == len(jobs) // 2 and qt + 1 < NT:
                                    qprep(qt + 1)
                                if g == 1 and i == len(jobs) // 2 and qt + 1 < NT:
                                    qprep_b(qt + 1)
                                if hm:
                                    mk, mrhs = jb['mask']
                                    if mk == 'id':
                                        P.op('pe', lambda e, jb=jb, sp_=sp_, mrhs=mrhs: e.matmul(sp_, lhsT=ident[:, 0:jb['nk']], rhs=mrhs, start=False, stop=True),
                                             reads=['ident', 'cst'], writes=[skey])
                                    else:
                                        P.op('pe', lambda e, jb=jb, sp_=sp_, mrhs=mrhs: e.matmul(sp_, lhsT=mrhs, rhs=negselT[:], start=False, stop=True),
                                             reads=['negselT', 'cst'], writes=[skey])
                tile_tail(int(os.environ.get("NQT", NT)) - 1)
                P.barrier()
                P.flush()
            if stage <= 4:
                return nc
        with contextlib.ExitStack() as pp:
            wq = sbt(pp, "wq", [128, 8, 2048], BF16)
            keysT = sbt(pp, "keysT", [128, 16, 128], BF16)
            x1b = [sbt(pp, "x1b%d" % i, [128, D], F32) for i in range(3)]
            junk2 = sbt(pp, "junk2", [128, D], BF16)
            ss2 = sbt(pp, "ss2", [128, 4], F32)
            h2f = sbt(pp, "h2f", [128, D], F32)
            ep_tmp = sbt(pp, "ep_tmp", [128, D], F32)
            h2b = [sbt(pp, "h2b%d" % i, [128, D], BF16) for i in range(2)]
            h2T = sbt(pp, "h2T", [128, 8, 128], BF16)
            qpT = sbt(pp, "qpT", [128, 16, 128], BF16)
            big8 = sbt(pp, "big8", [128, 2048], F32)
            s_sb = big8[:].rearrange("p (a b) -> p a b", b=128)
            oh = big8[:].rearrange("p (a b c) -> p a b c", b=16, c=16)
            sw = sbt(pp, "sw", [128, 128], F32)
            v16 = sbt(pp, "v16", [128, 16, 16], F32)
            i16 = sbt(pp, "i16", [128, 16, 16], U32)
            i16f = sbt(pp, "i16f", [128, 16, 16], F32)
            cand = sbt(pp, "cand", [128, 8, 256], F32)
            cw = sbt(pp, "cw", [128, 256], F32)
            tv = sbt(pp, "tv", [128, 8, 16], F32)
            pos = sbt(pp, "pos", [128, 8, 16], U32)
            posf = sbt(pp, "posf", [128, 8, 16], F32)
            posa = sbt(pp, "posa", [128, 8, 16], U32)
            posb_ = sbt(pp, "posb_", [128, 8, 16], U32)
            af_ = sbt(pp, "af_", [128, 8, 16], F32)
            bf_ = sbt(pp, "bf_", [128, 8, 16], F32)
            thr16 = sbt(pp, "thr16", [128, 16], F32)
            i1 = sbt(pp, "i1", [128, 8, 16], F32)
            i2 = sbt(pp, "i2", [128, 8, 16], F32)
            ef = sbt(pp, "ef", [128, 128], F32)
            eidx = [sbt(pp, "eidx%d" % i, [128, 128], U32) for i in range(2)]
            gw = [sbt(pp, "gw%d" % i, [128, 8, 16], F32) for i in range(2)]
            esum = sbt(pp, "esum", [128, 8], F32)
            actv = sbt(pp, "actv", [128, 128], F32)
            coef = sbt(pp, "coef", [128, 128], F32)
            NBUF = int(os.environ.get('NBUF', 16))
            GS = int(os.environ.get('GS', 4))
            uvb = [sbt(pp, "uvb%d" % i, [128, 2 * D], BF16) for i in range(NBUF)]
            prodb = [sbt(pp, "prodb%d" % i, [128, D], BF16) for i in range(4)]
            Dk4 = [sbt(pp, "Dk4_%d" % i, [128, 4, 128], BF16) for i in range(3)]
            coefb = sbt(pp, "coefb", [128, 128], BF16)

            wq_v = dr['peer_wq'].rearrange("(j p) c -> p j c", p=128)
            for q4 in range(4):
                P.dma('pool', lambda e, q4=q4: e.dma_start(out=wq[:, :, q4 * 512:(q4 + 1) * 512], in_=wq_v[:, :, q4 * 512:(q4 + 1) * 512]),
                      'wq%d' % q4, writes=[('wq', q4)])
            P.dma('pool', lambda e: e.dma_start(out=keysT[:], in_=dr['keysT']), 'wqk', writes=['keysT'])
            P.op('dve', lambda e: e.tensor_scalar(out=thr16[:], in0=iota16[:], scalar1=16.0, scalar2=16.0, op0=ALU.mult, op1=ALU.add),
                 reads=['iota16'], writes=['thr16'])

            def top16(T, src_ap, vout, iout, scratch, rk, wk):
                T(lambda: P.op('dve', lambda e: e.max(out=vout[:, 0:8], in_=src_ap), reads=rk, writes=[wk + 'v0']))
                T(lambda: P.op('dve', lambda e: e.max_index(out=iout[:, 0:8], in_max=vout[:, 0:8], in_values=src_ap), reads=rk + [wk + 'v0'], writes=[wk + 'i0']))
                T(lambda: P.op('dve', lambda e: e.match_replace(out=scratch, in_to_replace=vout[:, 0:8], in_values=src_ap, imm_value=-1e30),
                               reads=rk + [wk + 'v0'], writes=[wk + 'scr']))
                T(lambda: P.op('dve', lambda e: e.max(out=vout[:, 8:16], in_=scratch), reads=[wk + 'scr'], writes=[wk + 'v1']))
                T(lambda: P.op('dve', lambda e: e.max_index(out=iout[:, 8:16], in_max=vout[:, 8:16], in_values=scratch), reads=[wk + 'scr', wk + 'v1'], writes=[wk + 'i1']))
                return [wk + 'v0', wk + 'v1'], [wk + 'i0', wk + 'i1']

            def prologue(tt):
                th = []
                T = th.append
                b = tt % 2
                xb = tt % 3
                tsl = slice(tt * 128, (tt + 1) * 128)
                xt = x1b[xb]
                T(lambda: P.dma('sp', lambda e: e.dma_start(out=xt[:], in_=x1s[tsl, :]), 'x1i%d' % xb, reads=[('x1s', tt)], writes=[('x1b', xb)]))
                T(lambda: P.op('act', lambda e: e.activation(out=junk2[:], in_=xt[:], func=AF.Square, accum_out=ss2[:, 0:1]),
                               reads=[('x1b', xb)], writes=['junk2', 'ss2']))
                T(lambda: P.op('dve', lambda e: e.tensor_scalar(out=ss2[:, 1:2], in0=ss2[:, 0:1], scalar1=1.0 / D, scalar2=EPS, op0=ALU.mult, op1=ALU.add),
                               reads=['ss2'], writes=['ss2']))
                T(lambda: P.op('act', lambda e: e.activation(out=ss2[:, 2:3], in_=ss2[:, 1:2], func=AF.Sqrt), reads=['ss2'], writes=['ss2']))
                T(lambda: P.op('dve', lambda e: e.reciprocal(out=ss2[:, 3:4], in_=ss2[:, 2:3]), reads=['ss2'], writes=['ss2']))
                T(lambda: P.op('dve', lambda e: e.scalar_tensor_tensor(out=h2f[:], in0=xt[:], scalar=ss2[:, 3:4], in1=a_f, op0=ALU.mult, op1=ALU.mult),
                               reads=[('x1b', xb), 'ss2', 'a_f'], writes=['h2f']))
                T(lambda: P.op('dve', lambda e: e.tensor_tensor(out=h2b[b][:], in0=h2f[:], in1=sh_f, op=ALU.add), reads=['h2f'], writes=[('h2b', b)]))

                marks = {'pe_a': len(th)}

                def pe_a():
                    pT0 = psb[0][:].bitcast(BF16)
                    for j in range(8):
                        P.op('pe', lambda e, j=j: e.transpose(out=pT0[:, j * 128:(j + 1) * 128], in_=h2b[b][:, j * 128:(j + 1) * 128], identity=ident[:]),
                             reads=[('h2b', b), 'ident'], writes=[('ps', 0)])
                    P.op('act', lambda e: e.activation(out=h2T[:], in_=pT0.rearrange("p (a b) -> p a b", b=128), func=AF.Copy), reads=[('ps', 0)], writes=['h2T'])
                T(pe_a)
                marks['pe_b'] = len(th)

                def pe_b(q4, hc4):
                    pq_ = psb[(q4 + 1) % 2]
                    pk_ = ('ps', (q4 + 1) % 2)
                    hc = q4 * 4 + hc4
                    for j in range(8):
                        P.op('pe', lambda e, j=j: e.matmul(pq_[:, hc4 * 128:(hc4 + 1) * 128], lhsT=wq[:, j, hc * 128:(hc + 1) * 128],
                                                           rhs=h2T[:, j, :], start=(j == 0), stop=(j == 7)),
                             reads=[('wq', q4), 'h2T'], writes=[pk_])
                    if hc4 == 3:
                        P.op('act', lambda e: e.activation(out=qpT[:, q4 * 4:(q4 + 1) * 4, :], in_=pq_[:].rearrange("p (a b) -> p a b", b=128), func=AF.Copy),
                             reads=[pk_], writes=[('qpT', q4)])
                for q4 in range(4):
                    for hc4 in range(4):
                        T(lambda q4=q4, hc4=hc4: pe_b(q4, hc4))
                marks['pe_c'] = len(th)

                def pe_c(q4):
                    ps_ = psb[4 + q4 % 2]
                    for hc4 in range(4):
                        hc = q4 * 4 + hc4
                        P.op('pe', lambda e, hc=hc, hc4=hc4: e.matmul(ps_[:, hc4 * 128:(hc4 + 1) * 128], lhsT=qpT[:, hc, :], rhs=keysT[:, hc, :], start=True, stop=True),
                             reads=[('qpT', q4), 'keysT'], writes=[('ps', 4 + q4 % 2)])
                    P.op('act', lambda e: e.activation(out=s_sb[:, q4 * 4:(q4 + 1) * 4, :], in_=ps_[:].rearrange("p (a b) -> p a b", b=128), func=AF.Copy),
                         reads=[('ps', 4 + q4 % 2)], writes=['big8'])
                for q4 in range(4):
                    T(lambda q4=q4: pe_c(q4))
                marks['rest'] = len(th)
                vks, iks = [], []
                for hc in range(16):
                    a_, b_ = top16(T, s_sb[:, hc, :], v16[:, hc, :], i16[:, hc, :], sw[:], ['big8'], 't%d_' % hc)
                    vks += a_
                    iks += b_
                T(lambda: P.op('dve', lambda e: e.tensor_copy(out=i16f[:], in_=i16[:]), reads=iks, writes=['i16f']))
                v16v = v16[:].rearrange("p (h c) k -> p h c k", c=2)
                i16v = i16f[:].rearrange("p (h c) k -> p h c k", c=2)
                T(lambda: P.op('dve', lambda e: e.tensor_tensor(out=cand[:].rearrange("p h (a b) -> p h a b", b=16),
                                                                in0=v16v[:, :, 0, :].unsqueeze(3).broadcast_to([128, 8, 16, 16]),
                                                                in1=v16v[:, :, 1, :].unsqueeze(2).broadcast_to([128, 8, 16, 16]), op=ALU.add),
                               reads=vks, writes=['cand']))
                tks, pks = [], []
                for h in range(8):
                    a_, b_ = top16(T, cand[:, h, :], tv[:, h, :], pos[:, h, :], cw[:], ['cand'], 'c%d_' % h)
                    tks += a_
                    pks += b_
                T(lambda: P.op('dve', lambda e: e.tensor_scalar(out=posa[:], in0=pos[:], scalar1=4, scalar2=None, op0=ALU.logical_shift_right),
                               reads=pks, writes=['posa']))
                T(lambda: P.op('dve', lambda e: e.tensor_scalar(out=posb_[:], in0=pos[:], scalar1=15, scalar2=None, op0=ALU.bitwise_and),
                               reads=pks, writes=['posb_']))
                T(lambda: P.op('dve', lambda e: e.tensor_copy(out=af_[:], in_=posa[:]), reads=['posa'] + vks, writes=['af_']))
                T(lambda: P.op('dve', lambda e: e.tensor_copy(out=bf_[:], in_=posb_[:]), reads=['posb_'], writes=['bf_']))
                for (src, c_, dst, nm) in ((af_, 0, i1, 'i1'), (bf_, 1, i2, 'i2')):
                    T(lambda src=src: P.op('dve', lambda e: e.tensor_tensor(out=oh, in0=src[:].unsqueeze(3).broadcast_to([128, 8, 16, 16]),
                                                                            in1=iota16[:].unsqueeze(1).unsqueeze(1).broadcast_to([128, 8, 16, 16]), op=ALU.is_equal),
                                           reads=['af_', 'bf_', 'iota16'], writes=['big8']))
                    T(lambda c_=c_: P.op('dve', lambda e: e.tensor_tensor(out=oh, in0=oh, in1=i16v[:, :, c_, :].unsqueeze(2).broadcast_to([128, 8, 16, 16]), op=ALU.mult),
                                         reads=['big8', 'i16f'], writes=['big8']))
                    T(lambda dst=dst, nm=nm: P.op('dve', lambda e: e.tensor_reduce(out=dst[:], in_=oh, axis=AX.X, op=ALU.add), reads=['big8'], writes=[nm]))
                T(lambda: P.op('dve', lambda e: e.scalar_tensor_tensor(out=ef[:].rearrange("p (h k) -> p h k", k=16), in0=i1[:], scalar=128.0, in1=i2[:], op0=ALU.mult, op1=ALU.add),
                               reads=['i1', 'i2'], writes=['ef']))
                T(lambda: P.op('dve', lambda e: e.tensor_copy(out=eidx[b][:], in_=ef[:]), reads=['ef'], writes=[('eidx', b)]))
                gwb = gw[b]
                T(lambda: P.op('dve', lambda e: e.tensor_tensor(out=gwb[:], in0=tv[:], in1=tv[:, :, 0:1].broadcast_to([128, 8, 16]), op=ALU.subtract), reads=tks, writes=[('gw', b)]))
                T(lambda: P.op('act', lambda e: e.activation(out=gwb[:], in_=gwb[:], func=AF.Exp), reads=[('gw', b)], writes=[('gw', b)]))
                T(lambda: P.op('dve', lambda e: e.tensor_reduce(out=esum[:], in_=gwb[:], axis=AX.X, op=ALU.add), reads=[('gw', b)], writes=['esum']))
                T(lambda: P.op('dve', lambda e: e.reciprocal(out=esum[:], in_=esum[:]), reads=['esum'], writes=['esum']))
                T(lambda: P.op('dve', lambda e: e.tensor_tensor(out=gwb[:], in0=gwb[:], in1=esum[:].unsqueeze(2).broadcast_to([128, 8, 16]), op=ALU.mult),
                               reads=[('gw', b), 'esum'], writes=[('gw', b)]))
                return th, marks

            NPT = int(os.environ.get("NPT", NT))
            pend, _ = prologue(0)
            for f in pend:
                f()
            cnt = 0

            def make_sched(th, mk, ngrp):
                sc = {}
                f32 = ngrp / 32.0

                def put(g, f):
                    sc.setdefault(min(ngrp - 1, int(g * f32)), []).append(f)
                n_norm = mk['pe_a']
                norm_g = [0, 0, 1, 2, 3, 4, 4]
                for i in range(n_norm):
                    put(norm_g[i] if i < len(norm_g) else 4, th[i])
                put(6, th[mk['pe_a']])
                for i in range(mk['pe_b'], mk['pe_c']):
                    put(7 + (i - mk['pe_b']) // 8, th[i])
                for i in range(mk['pe_c'], mk['rest']):
                    put(10 + (i - mk['pe_c']) // 2, th[i])
                rest = th[mk['rest']:]
                g_lo, g_hi = 12, 31
                per_ = (len(rest) + (g_hi - g_lo) - 1) // (g_hi - g_lo)
                for i, f in enumerate(rest):
                    put(g_lo + i // per_, f)
                return sc

            def epilogue(tt):
                xb = tt % 3
                tsl = slice(tt * 128, (tt + 1) * 128)
                xt = x1b[xb]
                pob = 2 if tt % 2 == 0 else 6
                for half in range(2):
                    hs = slice(half * 512, (half + 1) * 512)
                    P.op('dve', lambda e, half=half, hs=hs: e.tensor_tensor(out=ep_tmp[:, hs], in0=psb[pob + half][:], in1=g_f[:, hs], op=ALU.mult),
                         reads=[('ps', pob + half)], writes=['ep_tmp'])
                    P.op('dve', lambda e, hs=hs: e.tensor_tensor(out=xt[:, hs], in0=ep_tmp[:, hs], in1=xt[:, hs], op=ALU.add),
                         reads=['ep_tmp', ('x1b', xb)], writes=[('x1b', xb)])
                P.dma('sp', lambda e: e.dma_start(out=out[tsl, :], in_=xt[:]), 'out%d' % xb, reads=[('x1b', xb)])

            dcnt = [0]
            assert GS <= 4
            ngrp = 128 // GS
            G0 = 2
            for tt in range(NPT):
                b = tt % 2
                if tt + 1 < NPT:
                    nth, nmk = prologue(tt + 1)
                    sched = make_sched(nth, nmk, ngrp)
                else:
                    sched = {}
                pob = 2 if tt % 2 == 0 else 6
                po = [psb[pob], psb[pob + 1]]
                slots = {}
                ti = 0
                for g in range(ngrp + 1):
                    if g < ngrp:
                        for k in range(GS):
                            hk = g * GS + k
                            sl_ = cnt % NBUF
                            slots[hk] = sl_
                            pr = prodb[cnt % 4]
                            cnt += 1
                            P.dma('pool', lambda e, hk=hk, sl_=sl_, b=b: e.indirect_dma_start(out=uvb[sl_][:], out_offset=None, in_=uvt,
                                                                                           in_offset=bass.IndirectOffsetOnAxis(ap=eidx[b][:, hk:hk + 1], axis=0)),
                                  'uv%d' % sl_, reads=[('eidx', b), 'uvt'], writes=[('uvb', sl_)])
                            P.op('dve', lambda e, sl_=sl_, pr=pr, b=b: e.tensor_tensor(out=pr[:], in0=uvb[sl_][:, 0:D], in1=h2b[b][:], op=ALU.mult),
                                 reads=[('uvb', sl_), ('h2b', b)], writes=[('prodb', id(pr))])
                            P.op('act', lambda e, hk=hk, pr=pr: e.activation(out=junk2[:], in_=pr[:], func=AF.Copy, accum_out=actv[:, hk:hk + 1]),
                                 reads=[('prodb', id(pr))], writes=['junk2', ('actv', g)])
                        gs = slice(g * GS, (g + 1) * GS)
                        P.op('act', lambda e, gs=gs: e.activation(out=coef[:, gs], in_=actv[:, gs], func=AF.Gelu), reads=[('actv', g)], writes=[('coef', g)])
                    if g >= 1:
                        g1 = g - 1
                        gs = slice(g1 * GS, (g1 + 1) * GS)
                        P.op('dve', lambda e, gs=gs, b=b: e.tensor_tensor(out=coefb[:, gs], in0=coef[:, gs], in1=gw[b][:].rearrange("p h k -> p (h k)")[:, gs], op=ALU.mult),
                             reads=[('coef', g1), ('gw', b)], writes=[('coefb', g1)])
                        dring = dcnt[0] % 3
                        dcnt[0] += 1
                        d4 = Dk4[dring]
                        P.op('dve', lambda e, gs=gs, d4=d4: e.tensor_tensor(out=d4[:, 0:GS, :], in0=ident[:].unsqueeze(1).broadcast_to([128, GS, 128]),
                                                                           in1=coefb[:, gs].unsqueeze(2).broadcast_to([128, GS, 128]), op=ALU.mult),
                             reads=['ident', ('coefb', g1)], writes=[('Dk4', dring)])
                        for k in range(GS):
                            hk = g1 * GS + k
                            sl_ = slots[hk]
                            for half in range(2):
                                P.op('pe', lambda e, hk=hk, d4=d4, k=k, sl_=sl_, half=half, po=po: e.matmul(po[half][:], lhsT=d4[:, k, :], rhs=uvb[sl_][:, D + half * 512:D + (half + 1) * 512],
                                                                                                           start=(hk == 0), stop=(hk == 127)),
                                     reads=[('Dk4', dring), ('uvb', sl_)], writes=[('ps', pob + half)])
                    if g == G0 and tt >= 1:
                        epilogue(tt - 1)
                    for f in sched.get(g, ()):
                        f()
            epilogue(NPT - 1)
            P.barrier()
            P.flush()
    return nc


def prep_inputs(inp):
    consts = host_constants()
    f = lambda a: np.ascontiguousarray(a, dtype=np.float32)
    shared = {}
    shared['w_ada'] = f(inp['w_ada'][0])
    shared['b_ada'] = f(inp['b_ada'][0][None, :])
    shared['ng_bc'] = f(np.broadcast_to(inp['norm_g'][0][None], (128, 2, D)))
    shared['w_in'] = f(inp['w_in'][0])
    shared['w_out'] = f(inp['w_out'][0])
    pk = inp['cmp_pe_k'][0]
    pv = inp['cmp_pe_v'][0]
    pe_tab = np.zeros((64, 4, 512), np.float32)
    for i, (p, lo) in enumerate([(pk, 0), (pk, 16), (pv, 0), (pv, 16)]):
        pe_tab[:, i, :] = np.tile(p[lo:lo + 16].T, (1, 32))
    shared['pe_tab'] = pe_tab
    shared['wck'] = f(inp['w_cmp_k'][0].transpose(1, 0, 2))
    shared['wcv'] = f(inp['w_cmp_v'][0].transpose(1, 0, 2))
    shared['qkg_bc'] = f(np.broadcast_to(inp['qk_norm_g'][0][None], (128, 4, 64)))
    shared['dwT'] = f(inp['dw_w'][0].reshape(31, 4, 128).transpose(2, 1, 0))
    cv = np.stack([inp['dw_b'][0], inp['conv_ln_g'][0], inp['conv_ln_b'][0]], 0)
    shared['cvec'] = f(cv.reshape(3, 4, 128).transpose(2, 0, 1))
    shared['peer_wq'] = f(inp['peer_wq'][0])
    shared['keysT'] = f(inp['peer_sub_keys'][0].reshape(16, 128, 128).transpose(2, 0, 1))
    shared['peer_u'] = f(inp['peer_u'][0])
    shared['peer_v'] = f(inp['peer_v'][0])
    shared.update(consts)
    maps = []
    for b in range(8):
        m = dict(shared)
        m['x'] = f(inp['x'][b])
        m['cT'] = f(inp['c'][b].reshape(8, 128).T)
        maps.append(m)
    return maps


def kernel(**inputs):
    maps = prep_inputs(inputs)
    nc = build()
    res = run_bass_kernel_spmd(nc, maps, core_ids=list(range(8)))
    return np.stack([r["out"] for r in res.results], axis=0).astype(np.float32)
```
